# Optimizing a Trainium2 kernel written in Bass

```python
import math
import jax, jax.numpy as jnp
from jax import lax
import numpy as np

D_MODEL = 1024
BATCH = 4
SEQ = 8192
DEPTH = 4

D_BRANCH = 512
M_HEADS = 4
M_HEAD_DIM = D_BRANCH // M_HEADS
M_CHUNK = 64
M_CONV = 4
R_BLOCKS = 8
R_BLOCK_DIM = D_BRANCH // R_BLOCKS
R_CONV = 4
R_C = 8.0
A_HEADS = 8
A_HEAD_DIM = D_BRANCH // A_HEADS
IDX_HEADS = 8
IDX_DIM = 64
TOPK_MAX = 256
Q_BLOCK = 128
ROPE_THETA = 10000.0
D_FF = 3 * D_MODEL
FFN_CONV = 3
EPS = 1e-6

IN_SPLITS = (
    2 * D_BRANCH,
    D_BRANCH,
    D_BRANCH,
    M_HEADS,
    M_HEADS,
    D_BRANCH,
    D_BRANCH,
    D_BRANCH,
    D_BRANCH,
    D_BRANCH,
    IDX_HEADS * IDX_DIM,
    IDX_DIM,
    IDX_HEADS,
    3 * D_MODEL,
)
N_IN = sum(IN_SPLITS)

kernel_name = 'hybrid_mlstm_rglru_dsa_trunk'


def rms_norm(x, g):
    xf = x.astype(jnp.float32)
    y = xf * lax.rsqrt(jnp.mean(xf * xf, axis=-1, keepdims=True) + EPS)
    return (y * g.astype(jnp.float32)).astype(x.dtype)


def causal_dwconv(x, w, b):
    K, C = w.shape
    y = lax.conv_general_dilated(x, w[:, None, :].astype(x.dtype), window_strides=(1,),
                                 padding=[(K - 1, 0)], dimension_numbers=('NWC', 'WIO', 'NWC'),
                                 feature_group_count=C)
    return y + b.astype(x.dtype)


def rope_tables(positions, dim):
    inv = ROPE_THETA ** (-jnp.arange(0, dim, 2, dtype=jnp.float32) / dim)
    ang = positions.astype(jnp.float32)[..., None] * inv
    return jnp.cos(ang), jnp.sin(ang)


def apply_rope(x, cos, sin):
    x1, x2 = jnp.split(x.astype(jnp.float32), 2, axis=-1)
    c = cos[:, :, None, :]
    s = sin[:, :, None, :]
    return jnp.concatenate([x1 * c - x2 * s, x2 * c + x1 * s], axis=-1).astype(x.dtype)


def head_layer_norm(h, g):
    B, S, H, Dh = h.shape
    mu = jnp.mean(h, axis=-1, keepdims=True)
    var = jnp.mean(jnp.square(h - mu), axis=-1, keepdims=True)
    y = (h - mu) * lax.rsqrt(var + EPS)
    return y.reshape(B, S, H * Dh) * g.astype(jnp.float32)


def mlstm_chunkwise(q, k, v, i_pre, f_pre):
    B, S, H, Dh = q.shape
    L = M_CHUNK
    nc = S // L
    f32 = jnp.float32

    def to_chunks(t):
        return t.astype(f32).reshape(B, nc, L, H, Dh).transpose(1, 0, 3, 2, 4)

    def gate_chunks(t):
        return t.astype(f32).reshape(B, nc, L, H).transpose(1, 0, 3, 2)

    qc = to_chunks(q)
    kc = to_chunks(k) * (Dh ** -0.5)
    vc = to_chunks(v)
    ic = gate_chunks(i_pre)
    gc = jnp.cumsum(jax.nn.log_sigmoid(gate_chunks(f_pre)), axis=-1)
    causal = jnp.tril(jnp.ones((L, L), dtype=bool))

    def step(carry, inp):
        C, n, m = carry
        qb, kb, vb, ib, gb = inp
        logd = jnp.where(causal, gb[..., :, None] - gb[..., None, :] + ib[..., None, :], -jnp.inf)
        log_inter = gb + m[..., None]
        m_t = jnp.maximum(log_inter, jnp.max(logd, axis=-1))
        s = jnp.einsum('bhtd,bhsd->bhts', qb, kb) * jnp.exp(logd - m_t[..., None])
        w_inter = jnp.exp(log_inter - m_t)
        num = jnp.einsum('bhts,bhsd->bhtd', s, vb) + w_inter[..., None] * jnp.einsum('bhvk,bhtk->bhtv', C, qb)
        den = jnp.sum(s, axis=-1) + w_inter * jnp.einsum('bhk,bhtk->bht', n, qb)
        h = num / jnp.maximum(jnp.abs(den), jnp.exp(-m_t))[..., None]
        g_last = gb[..., -1]
        m_new = m_t[..., -1]
        w_state = jnp.exp(g_last[..., None] - gb + ib - m_new[..., None])
        decay = jnp.exp(g_last + m - m_new)
        C = decay[..., None, None] * C + jnp.einsum('bhs,bhsv,bhsk->bhvk', w_state, vb, kb)
        n = decay[..., None] * n + jnp.einsum('bhs,bhsk->bhk', w_state, kb)
        return (C, n, m_new), h

    init = (jnp.zeros((B, H, Dh, Dh), f32), jnp.zeros((B, H, Dh), f32), jnp.zeros((B, H), f32))
    _, hs = lax.scan(step, init, (qc, kc, vc, ic, gc))
    return hs.transpose(1, 0, 3, 2, 4).reshape(B, S, H, Dh)


def rg_lru(x, w_a, b_a, w_x, b_x, lam):
    B, S, C = x.shape
    xf = x.astype(jnp.float32)
    xb = xf.reshape(B, S, R_BLOCKS, R_BLOCK_DIM)
    r = jax.nn.sigmoid(jnp.einsum('bsnd,nde->bsne', xb, w_a.astype(jnp.float32)).reshape(B, S, C) + b_a.astype(jnp.float32))
    i = jax.nn.sigmoid(jnp.einsum('bsnd,nde->bsne', xb, w_x.astype(jnp.float32)).reshape(B, S, C) + b_x.astype(jnp.float32))
    log_a = -R_C * r * jax.nn.softplus(-lam.astype(jnp.float32))
    a = jnp.exp(log_a)
    u = jnp.sqrt(-jnp.expm1(2.0 * log_a)) * (i * xf)

    def combine(left, right):
        a1, b1 = left
        a2, b2 = right
        return a1 * a2, a2 * b1 + b2

    _, h = lax.associative_scan(combine, (a, u), axis=1)
    return h.astype(x.dtype)


def dsa_attention(q, k, v, q_idx, k_idx, w_idx):
    B, S, H, Dh = q.shape
    f32 = jnp.float32
    top_k = min(TOPK_MAX, S // 4)
    nb = S // Q_BLOCK
    key_pos = jnp.arange(S, dtype=jnp.int32)
    k_idx_f = k_idx.astype(f32)

    def blocks(t):
        return jnp.moveaxis(t.reshape((B, nb, Q_BLOCK) + t.shape[2:]), 1, 0)

    def attend(args):
        qb, qib, wb, start = args
        qpos = start + jnp.arange(Q_BLOCK, dtype=jnp.int32)
        visible = key_pos[None, None, :] <= qpos[None, :, None]
        logits = jnp.einsum('bqhd,bsd->bqhs', qib.astype(f32), k_idx_f) * (IDX_DIM ** -0.5)
        score = jnp.einsum('bqh,bqhs->bqs', wb.astype(f32) * (IDX_HEADS ** -0.5), jax.nn.relu(logits))
        score = jnp.where(visible, score, -jnp.inf)
        _, idx = lax.top_k(score, top_k)
        k_sel = jax.vmap(lambda kk, ii: kk[ii])(k, idx)
        v_sel = jax.vmap(lambda vv, ii: vv[ii])(v, idx)
        att = jnp.einsum('bqhd,bqkhd->bqhk', qb.astype(f32), k_sel.astype(f32)) * (Dh ** -0.5)
        keep = (idx <= qpos[None, :, None])[:, :, None, :]
        p = jax.nn.softmax(jnp.where(keep, att, -jnp.inf), axis=-1)
        return jnp.einsum('bqhk,bqkhd->bqhd', p, v_sel.astype(f32)).astype(q.dtype)

    starts = jnp.arange(nb, dtype=jnp.int32) * Q_BLOCK
    out = lax.map(attend, (blocks(q), blocks(q_idx), blocks(w_idx), starts))
    return jnp.moveaxis(out, 0, 1).reshape(B, S, H, Dh)


def setup_inputs(seed: int = 0) -> dict:
    key = jax.random.key(seed)
    ks = jax.random.split(key, 24)
    f32 = jnp.float32

    def normal(k, shape, scale):
        return jax.random.normal(k, shape, f32) * scale

    def gain(k, shape):
        return 1.0 + 0.02 * jax.random.normal(k, shape, f32)

    x = normal(ks[0], (BATCH, SEQ, D_MODEL), 1.0)
    positions = (jnp.arange(SEQ, dtype=jnp.int32)[None, :]
                 + jax.random.randint(ks[1], (BATCH, 1), 0, 4096, dtype=jnp.int32))
    norm_mix = gain(ks[2], (DEPTH, D_MODEL))
    w_in = normal(ks[3], (DEPTH, D_MODEL, N_IN), D_MODEL ** -0.5)
    mlstm_conv_w = normal(ks[4], (DEPTH, M_CONV, 2 * D_BRANCH), M_CONV ** -0.5)
    mlstm_conv_b = normal(ks[5], (DEPTH, 2 * D_BRANCH), 0.02)
    mlstm_i_bias = normal(ks[6], (DEPTH, M_HEADS), 0.1)
    mlstm_f_bias = jnp.linspace(3.0, 6.0, M_HEADS, dtype=f32)[None, :] + normal(ks[7], (DEPTH, M_HEADS), 0.1)
    mlstm_norm = gain(ks[8], (DEPTH, D_BRANCH))
    rglru_conv_w = normal(ks[9], (DEPTH, R_CONV, D_BRANCH), R_CONV ** -0.5)
    rglru_conv_b = normal(ks[10], (DEPTH, D_BRANCH), 0.02)
    rglru_w_a = normal(ks[11], (DEPTH, R_BLOCKS, R_BLOCK_DIM, R_BLOCK_DIM), R_BLOCK_DIM ** -0.5)
    rglru_b_a = normal(ks[12], (DEPTH, D_BRANCH), 0.02)
    rglru_w_x = normal(ks[13], (DEPTH, R_BLOCKS, R_BLOCK_DIM, R_BLOCK_DIM), R_BLOCK_DIM ** -0.5)
    rglru_b_x = normal(ks[14], (DEPTH, D_BRANCH), 0.02)
    a_c = jax.random.uniform(ks[15], (DEPTH, D_BRANCH), f32, 0.9, 0.999)
    sig = a_c ** (1.0 / R_C)
    rglru_lambda = jnp.log(sig) - jnp.log1p(-sig)
    w_branch = normal(ks[16], (DEPTH, 3, D_BRANCH, D_MODEL), D_BRANCH ** -0.5)
    w_out = normal(ks[17], (DEPTH, D_MODEL, D_MODEL), D_MODEL ** -0.5)
    norm_ffn = gain(ks[18], (DEPTH, D_MODEL))
    ffn_up = normal(ks[19], (DEPTH, D_MODEL, 2 * D_FF), D_MODEL ** -0.5)
    ffn_conv_w = normal(ks[20], (DEPTH, FFN_CONV, D_FF), FFN_CONV ** -0.5)
    ffn_conv_b = normal(ks[21], (DEPTH, D_FF), 0.02)
    ffn_down = normal(ks[22], (DEPTH, D_FF, D_MODEL), D_FF ** -0.5)
    norm_final = gain(ks[23], (D_MODEL,))
    return {'x': x, 'positions': positions, 'norm_mix': norm_mix, 'w_in': w_in,
            'mlstm_conv_w': mlstm_conv_w, 'mlstm_conv_b': mlstm_conv_b,
            'mlstm_i_bias': mlstm_i_bias, 'mlstm_f_bias': mlstm_f_bias, 'mlstm_norm': mlstm_norm,
            'rglru_conv_w': rglru_conv_w, 'rglru_conv_b': rglru_conv_b,
            'rglru_w_a': rglru_w_a, 'rglru_b_a': rglru_b_a, 'rglru_w_x': rglru_w_x, 'rglru_b_x': rglru_b_x,
            'rglru_lambda': rglru_lambda, 'w_branch': w_branch, 'w_out': w_out, 'norm_ffn': norm_ffn,
            'ffn_up': ffn_up, 'ffn_conv_w': ffn_conv_w, 'ffn_conv_b': ffn_conv_b, 'ffn_down': ffn_down,
            'norm_final': norm_final}


def reference(x, positions, norm_mix, w_in, mlstm_conv_w, mlstm_conv_b, mlstm_i_bias, mlstm_f_bias,
              mlstm_norm, rglru_conv_w, rglru_conv_b, rglru_w_a, rglru_b_a, rglru_w_x, rglru_b_x,
              rglru_lambda, w_branch, w_out, norm_ffn, ffn_up, ffn_conv_w, ffn_conv_b, ffn_down,
              norm_final):
    B, S, _ = x.shape
    cos, sin = rope_tables(positions, A_HEAD_DIM)
    split_at = np.cumsum(IN_SPLITS)[:-1].tolist()
    m_shape = (B, S, M_HEADS, M_HEAD_DIM)
    a_shape = (B, S, A_HEADS, A_HEAD_DIM)
    for l in range(DEPTH):
        h = rms_norm(x, norm_mix[l])
        (m_qk, m_v, m_o, m_i, m_f, r_x, r_g, a_q, a_k, a_v, a_qi, a_ki, a_w, gates) = jnp.split(
            h @ w_in[l], split_at, axis=-1)
        m_qk = jax.nn.silu(causal_dwconv(m_qk, mlstm_conv_w[l], mlstm_conv_b[l]))
        m_q, m_k = jnp.split(m_qk, 2, axis=-1)
        h_m = mlstm_chunkwise(m_q.reshape(m_shape), m_k.reshape(m_shape), m_v.reshape(m_shape),
                              m_i + mlstm_i_bias[l], m_f + mlstm_f_bias[l])
        y_a = (head_layer_norm(h_m, mlstm_norm[l]) * jax.nn.sigmoid(m_o.astype(jnp.float32))).astype(x.dtype)
        r_x = causal_dwconv(r_x, rglru_conv_w[l], rglru_conv_b[l])
        y_b = jax.nn.gelu(r_g) * rg_lru(r_x, rglru_w_a[l], rglru_b_a[l], rglru_w_x[l], rglru_b_x[l], rglru_lambda[l])
        q_c = apply_rope(a_q.reshape(a_shape), cos, sin)
        k_c = apply_rope(a_k.reshape(a_shape), cos, sin)
        qi = apply_rope(a_qi.reshape(B, S, IDX_HEADS, IDX_DIM), cos, sin)
        ki = apply_rope(a_ki[:, :, None, :], cos, sin)[:, :, 0, :]
        y_c = dsa_attention(q_c, k_c, a_v.reshape(a_shape), qi, ki, a_w).reshape(B, S, D_BRANCH)
        g_a, g_b, g_c = jnp.split(jax.nn.sigmoid(gates), 3, axis=-1)
        merged = (g_a * (y_a @ w_branch[l, 0]) + g_b * (y_b @ w_branch[l, 1])
                  + g_c * (y_c @ w_branch[l, 2]))
        x = x + merged @ w_out[l]
        h = rms_norm(x, norm_ffn[l])
        ff_g, ff_u = jnp.split(h @ ffn_up[l], 2, axis=-1)
        ff_g = causal_dwconv(ff_g, ffn_conv_w[l], ffn_conv_b[l])
        x = x + (jax.nn.gelu(ff_g) * ff_u) @ ffn_down[l]
    return rms_norm(x, norm_final)
```

```python
import contextlib
import math
import numpy as np
import ml_dtypes
import concourse.bass as bass
import concourse.mybir as mybir
from concourse.bass_utils import run_bass_kernel_spmd
from concourse.alu_op_type import AluOpType as ALU

F32 = mybir.dt.float32
BF16 = mybir.dt.bfloat16
I32 = mybir.dt.int32
AF = mybir.ActivationFunctionType
AX = mybir.AxisListType

D = 1024
DB = 512
DFF = 3072
EPS = 1e-6
ENGS = ['pe', 'act', 'dve', 'pool', 'sp']
NBIS = 12
import os
SAFE_SAME_ENGINE = os.environ.get('SAFE_SAME', '1') == '1'


class Buf:
    __slots__ = ('t', 'w', 'r', 'dsem', 'dcnt', 'name')

    def __init__(self, t, name):
        self.t = t
        self.w = None
        self.r = {}
        self.dsem = None
        self.dcnt = 0
        self.name = name

    def __getitem__(self, k):
        return self.t[k]


class Prog:
    def __init__(self, nc):
        self.nc = nc
        self.engs = {'pe': nc.tensor, 'act': nc.scalar, 'dve': nc.vector, 'pool': nc.gpsimd, 'sp': nc.sync}
        self.sem = {e: nc.alloc_semaphore(name='s_' + e) for e in ENGS}
        self.cnt = {e: 0 for e in ENGS}
        self.known = {e: {} for e in ENGS}
        self.bufs = []
        self.dbufs = []
        self.pool = []
        for i in range(120):
            try:
                self.pool.append([nc.alloc_semaphore(name=f'dq{i}'), 0])
            except Exception:
                break
        self.free = list(range(len(self.pool)))
        self.ninst = 0
        self.nwait = 0

    def sb(self, es, name, shape, dt):
        self.uid = getattr(self, 'uid', 0) + 1
        name = f"{name}_{self.uid}"
        t = es.enter_context(self.nc.sbuf_tensor(name, list(shape), dt))
        b = Buf(t, name)
        self.bufs.append(b)
        return b

    def wrap(self, t, name):
        b = Buf(t, name)
        self.bufs.append(b)
        return b

    def _waits(self, e, deps):
        eng = self.engs[e]
        kn = self.known[e]
        for key, sem, val in deps:
            if kn.get(key, 0) >= val:
                continue
            kn[key] = val
            eng.wait_ge(sem, val)
            self.nwait += 1

    def _deps(self, e, reads, writes):
        deps = []
        for b in reads:
            if b.w is not None:
                deps.append(b.w)
        for b in writes:
            if b.w is not None:
                deps.append(b.w)
            deps.extend(b.r.values())
        if e == 'pe' or not SAFE_SAME_ENGINE:
            deps = [d for d in deps if d[0] != e]
        return deps

    def op(self, e, fn, reads=(), writes=()):
        self._waits(e, self._deps(e, reads, writes))
        self.cnt[e] += 1
        tok = (e, self.sem[e], self.cnt[e])
        fn(self.engs[e]).then_inc(self.sem[e], 1)
        self.ninst += 1
        for b in reads:
            b.r[e] = tok
        for b in writes:
            b.w = tok
            b.r = {}

    def dma(self, q, out_ap, in_ap, sbuf, reads=(), writes=()):
        self._waits(q, self._deps(q, reads, writes))
        if sbuf.dsem is None:
            sbuf.dsem = self.free.pop()
            self.dbufs.append(sbuf)
        pe_ = self.pool[sbuf.dsem]
        pe_[1] += 16
        key = ('d', sbuf.dsem)
        tok = (key, pe_[0], pe_[1])
        self.engs[q].dma_start(out=out_ap, in_=in_ap).then_inc(pe_[0], 16)
        self.ninst += 1
        for b in reads:
            b.r[key] = tok
        for b in writes:
            b.w = tok
            b.r = {}

    def load(self, buf, out_ap, in_ap, q='sp'):
        self.dma(q, out_ap, in_ap, buf, writes=(buf,))

    def store(self, buf, out_ap, in_ap, q='sp'):
        self.dma(q, out_ap, in_ap, buf, reads=(buf,))

    def barrier(self):
        toks = [(e, self.sem[e], self.cnt[e]) for e in ENGS if self.cnt[e] > 0]
        toks += [(('d', i), pe_[0], pe_[1]) for i, pe_ in enumerate(self.pool) if pe_[1] > 0]
        for e in ENGS:
            self._waits(e, [t for t in toks if t[0] != e])
        for b in self.bufs:
            b.w = None
            b.r = {}
        for b in self.dbufs:
            self.free.append(b.dsem)
            b.dsem = None
        self.dbufs = []


def MM(P, out_b, out_ap, lhs_b, lhsT, rhs_b, rhs, start, stop):
    P.op('pe', lambda e: e.matmul(out_ap, lhsT=lhsT, rhs=rhs, start=start, stop=stop),
         reads=(lhs_b, rhs_b), writes=(out_b,))


def TR(P, out_b, out_ap, in_b, in_ap, id_b, ident):
    P.op('pe', lambda e: e.transpose(out_ap, in_ap, ident), reads=(in_b, id_b), writes=(out_b,))


def ACT(P, out_b, out_ap, in_b, in_ap, func, bias=None, scale=None, accum=None, extra_r=(), extra_w=(), eng='act'):
    kw = {}
    if bias is not None:
        kw['bias'] = bias
    if scale is not None:
        kw['scale'] = scale
    if accum is not None:
        kw['accum_out'] = accum
    P.op('act', lambda e: e.activation(out=out_ap, in_=in_ap, func=func, **kw),
         reads=(in_b,) + tuple(extra_r), writes=(out_b,) + tuple(extra_w))


def TS(P, eng, out_b, out_ap, in_b, in_ap, s1, s2, op0, op1=None, accum=None, extra_r=(), extra_w=()):
    kw = {}
    if op1 is not None:
        kw['op1'] = op1
    if accum is not None:
        kw['accum_out'] = accum
    P.op(eng, lambda e: e.tensor_scalar(out=out_ap, in0=in_ap, scalar1=s1, scalar2=s2, op0=op0, **kw),
         reads=(in_b,) + tuple(extra_r), writes=(out_b,) + tuple(extra_w))


def TT(P, eng, out_b, out_ap, a_b, a_ap, b_b, b_ap, op):
    P.op(eng, lambda e: e.tensor_tensor(out=out_ap, in0=a_ap, in1=b_ap, op=op), reads=(a_b, b_b), writes=(out_b,))


def STT(P, out_b, out_ap, a_b, a_ap, scalar, b_b, b_ap, op0, op1, extra_r=()):
    P.op('dve', lambda e: e.scalar_tensor_tensor(out=out_ap, in0=a_ap, scalar=scalar, in1=b_ap, op0=op0, op1=op1),
         reads=(a_b, b_b) + tuple(extra_r), writes=(out_b,))


def CP(P, eng, out_b, out_ap, in_b, in_ap):
    if eng == 'act':
        P.op('act', lambda e: e.activation(out=out_ap, in_=in_ap, func=AF.Copy), reads=(in_b,), writes=(out_b,))
    else:
        P.op(eng, lambda e: e.tensor_copy(out=out_ap, in_=in_ap), reads=(in_b,), writes=(out_b,))


def MEMSET(P, eng, b, ap, val):
    P.op(eng, lambda e: e.memset(ap, val), writes=(b,))


SPL = np.cumsum([0, 1024, 512, 512, 4, 4, 512, 512, 512, 512, 512, 512, 64, 8, 3072])
NA = 6656 + 128 + 1536


def perm_w_in():
    s = SPL
    r = lambda a, b: list(range(a, b))
    fm = r(s[0], s[1]) + r(s[5], s[6]) + r(s[6], s[7]) + r(s[7], s[8]) + r(s[8], s[9]) + r(s[10], s[11]) + r(s[13], s[14])
    small = r(s[11], s[12]) + r(s[3], s[4]) + r(s[4], s[5]) + r(s[12], s[13])
    tm = r(s[1], s[2]) + r(s[2], s[3]) + r(s[9], s[10])
    return fm, small, tm


def build(S, depth, dbg=(), stop=99):
    nc = bass.Bass("TRN2", target_bir_lowering=False)
    NC = S // 128
    NT = S // 512
    P = Prog(nc)
    outs = {}

    def din(name, shape, dt=F32):
        return nc.dram_tensor(name, list(shape), dt, kind="ExternalInput").ap()

    def dscr(name, shape, dt):
        kind = "ExternalOutput" if name in dbg else "Internal"
        return nc.dram_tensor(name, list(shape), dt, kind=kind).ap()

    x_in = din("x", [S, D])
    pos_in = din("pos", [1, S], I32)
    wA_in = din("wA", [depth, D, NA])
    gmix_in = din("gmix", [depth, 128, 8])
    gffn_in = din("gffn", [depth, 128, 8])
    gfin_in = din("gfin", [128, 8])
    mcw_in = din("mcw", [depth, 8, 128, 4])
    mcb_in = din("mcb", [depth, 8, 128, 1])
    mib_in = din("mib", [depth, 4, 1])
    mfb_in = din("mfb", [depth, 4, 1])
    mng_in = din("mng", [depth, 4, 128])
    rcw_in = din("rcw", [depth, 4, 128, 4])
    rcb_in = din("rcb", [depth, 4, 128, 1])
    rwa_in = din("rwa", [depth, 4, 128, 128])
    rwx_in = din("rwx", [depth, 4, 128, 128])
    rba_in = din("rba", [depth, 4, 128, 1])
    rbx_in = din("rbx", [depth, 4, 128, 1])
    rlam_in = din("rlam", [depth, 4, 128, 1])
    wbr_in = din("wbr", [depth, 1536, D])
    wo_in = din("wo", [depth, D, D])
    fup_in = din("fup", [depth, D, 2 * DFF])
    fcw_in = din("fcw", [depth, 128, 24, 3])
    fcb_in = din("fcb", [depth, 128, 24])
    fdn_in = din("fdn", [depth, DFF, D])
    c_idf = din("c_idf", [128, 128])
    c_idb = din("c_idb", [128, 128], BF16)
    c_ones = din("c_ones", [128, 128], BF16)
    c_maskT = din("c_maskT", [128, 128], BF16)
    c_negm = din("c_negm", [128, 128])
    c_rot = din("c_rot", [128, 128], BF16)
    c_id240 = din("c_id240", [128, 128], BF16)
    c_sel8 = din("c_sel8", [8, 4, 128])
    c_selh = din("c_selh", [4, 4, 128])
    c_cvec = din("c_cvec", [128, 16])
    c_invf = din("c_invf", [128, 1])
    out_d = nc.dram_tensor("out", [S, D], F32, kind="ExternalOutput").ap()

    xT = dscr("xT", [D, S], F32)
    wA_b = dscr("wA_b", [D, NA], BF16)
    wbr_b = dscr("wbr_b", [1536, D], BF16)
    wo_b = dscr("wo_b", [D, D], BF16)
    fup_b = dscr("fup_b", [D, 2 * DFF], BF16)
    fdn_b = dscr("fdn_b", [DFF, D], BF16)
    fmT = dscr("fmT", [6656, S], BF16)
    smT = dscr("smT", [128, S], F32)
    tmv = dscr("tmv", [S, 1536], BF16)
    yT = dscr("yT", [1536, S], BF16)
    cosT = dscr("cosT", [128, S], F32)
    sinT = dscr("sinT", [128, S], F32)
    qrT = dscr("qrT", [512, S], BF16)
    qiT = dscr("qiT", [512, S], BF16)
    kiT = dscr("kiT", [64, S], BF16)
    kblk = dscr("kblk", [NC, 128, 4, 128], BF16)
    vblk = dscr("vblk", [NC, 128, 8, 128], BF16)
    sgn_d = dscr("sgn_d", [S, 8], F32)
    FM_QK, FM_RX, FM_RG, FM_AQ, FM_AK, FM_QI, FM_G = 0, 1024, 1536, 2048, 2560, 3072, 3584

    with contextlib.ExitStack() as ges:
        idf = P.sb(ges, "idf", [128, 128], F32)
        idb = P.sb(ges, "idb", [128, 128], BF16)
        onesb = P.sb(ges, "onesb", [128, 128], BF16)
        maskT = P.sb(ges, "maskT", [128, 128], BF16)
        negm = P.sb(ges, "negm", [128, 128], F32)
        rot = P.sb(ges, "rot", [128, 128], BF16)
        id240 = P.sb(ges, "id240", [128, 128], BF16)
        sel8 = P.sb(ges, "sel8", [8, 4, 128], F32)
        selh = P.sb(ges, "selh", [4, 4, 128], F32)
        cvec = P.sb(ges, "cvec", [128, 16], F32)
        invf = P.sb(ges, "invf", [128, 1], F32)
        epsc = P.sb(ges, "epsc", [128, 1], F32)
        onec = P.sb(ges, "onec", [128, 1], F32)
        for b, src in ((idf, c_idf), (idb, c_idb), (onesb, c_ones), (maskT, c_maskT), (negm, c_negm), (rot, c_rot), (id240, c_id240),
                       (sel8, c_sel8), (selh, c_selh), (cvec, c_cvec), (invf, c_invf)):
            P.load(b, b[:], src)
        MEMSET(P, 'dve', epsc, epsc[:], EPS)
        MEMSET(P, 'dve', onec, onec[:], 1.0)
        psum_t = ges.enter_context(nc.psum_tensor("ps", [128, 8, 512], F32))
        PS = [P.wrap(psum_t, f"ps{i}") for i in range(8)]

        def bank(i):
            return PS[i], psum_t[:, i, :]

        def bank2(i):
            return PS[i], psum_t[:, i:i + 2, :]

        def stage_transpose_in():
            with contextlib.ExitStack() as es:
                xin = [P.sb(es, f"xin{i}", [128, D], F32) for i in range(2)]
                xo = [P.sb(es, f"xo{i}", [128, 8, 128], F32) for i in range(2)]
                for i in range(NC):
                    a = xin[i % 2]
                    o = xo[i % 2]
                    P.load(a, a[:], x_in[i * 128:(i + 1) * 128, :])
                    for hlf in range(2):
                        pb, pa = bank(2 * (i % 2) + hlf)
                        for c in range(4):
                            cc = hlf * 4 + c
                            TR(P, pb, pa[:, c * 128:(c + 1) * 128], a, a[:, cc * 128:(cc + 1) * 128], idf, idf[:])
                        CP(P, 'dve' if hlf == 0 else 'act', o, o[:, hlf * 4:(hlf + 1) * 4, :], pb,
                           pa.rearrange("p (c t) -> p c t", c=4))
                    P.store(o, xT[:, i * 128:(i + 1) * 128].rearrange("(c p) t -> p c t", p=128), o[:])
                P.barrier()

        def convert(es_name, src, dst, K, N, gsrc=None):
            with contextlib.ExitStack() as es:
                W = 2048
                a = [P.sb(es, f"cv_a{i}", [128, W], F32) for i in range(3)]
                o = [P.sb(es, f"cv_o{i}", [128, W], BF16) for i in range(3)]
                g = None
                if gsrc is not None:
                    g = P.sb(es, "cv_g", [128, K // 128], F32)
                    P.load(g, g[:], gsrc)
                k = 0
                for kc in range(K // 128):
                    for n0 in range(0, N, W):
                        w = min(W, N - n0)
                        ai, oi = a[k % 3], o[k % 3]
                        P.load(ai, ai[:, :w], src[kc * 128:(kc + 1) * 128, n0:n0 + w])
                        eng = 'act' if k % 2 == 0 else 'dve'
                        if g is None:
                            CP(P, eng, oi, oi[:, :w], ai, ai[:, :w])
                        elif eng == 'act':
                            ACT(P, oi, oi[:, :w], ai, ai[:, :w], AF.Copy, scale=g[:, kc:kc + 1], extra_r=(g,))
                        else:
                            TS(P, 'dve', oi, oi[:, :w], ai, ai[:, :w], g[:, kc:kc + 1], None, ALU.mult, extra_r=(g,))
                        P.store(oi, dst[kc * 128:(kc + 1) * 128, n0:n0 + w], oi[:, :w])
                        k += 1
                P.barrier()

        def rmsnorm_tile(es_bufs, t0, T, gcol=None):
            xt, sq, hT, rstd = es_bufs
            P.load(xt, xt[:, :, :T], xT[:, t0:t0 + T].rearrange("(c p) t -> p c t", p=128))
            ACT(P, sq, sq[:, :, :T], xt, xt[:, :, :T], AF.Square)
            for hf in range(T // 512):
                pb, pa = bank(6 + hf % 2)
                for c in range(8):
                    MM(P, pb, pa, onesb, onesb[:], sq, sq[:, c, hf * 512:(hf + 1) * 512], c == 0, c == 7)
                ACT(P, rstd, rstd[:, hf * 512:(hf + 1) * 512], pb, pa, AF.Sqrt, bias=epsc[:], scale=1.0 / D, extra_r=(epsc,))
            P.op('dve', lambda e: e.reciprocal(rstd[:, :T], rstd[:, :T]), reads=(rstd,), writes=(rstd,))
            if gcol is None:
                TT(P, 'dve', hT, hT[:, :, :T], xt, xt[:, :, :T], rstd, rstd[:, :T].unsqueeze(1).to_broadcast([128, 8, T]), ALU.mult)
            else:
                for c in range(8):
                    STT(P, hT, hT[:, c, :T], xt, xt[:, c, :T], gcol[:, c:c + 1], rstd, rstd[:, :T], ALU.mult, ALU.mult, extra_r=(gcol,))
            return xt, hT

        def stage_inproj():
            T = 1024 if S >= 1024 else S
            with contextlib.ExitStack() as es:
                nb = (P.sb(es, "A_xt", [128, 8, T], F32), P.sb(es, "A_sq", [128, 8, T], BF16),
                      P.sb(es, "A_hT", [128, 8, T], BF16), P.sb(es, "A_rstd", [128, T], F32))
                wb = [P.sb(es, f"A_w{i}", [128, 8, 512], BF16) for i in range(3)]
                ob = [P.sb(es, f"A_o{i}", [128, T], BF16) for i in range(4)]
                osm = P.sb(es, "A_osm", [128, T], F32)
                otm = [P.sb(es, f"A_ot{i}", [128, 512], BF16) for i in range(3)]
                nh = T // 512
                wk = 0
                ok = 0
                pk = 0
                for t0 in range(0, S, T):
                    xt, hT = rmsnorm_tile(nb, t0, T)
                    for blk in range(13):
                        w = wb[wk % 3]
                        wk += 1
                        P.load(w, w[:], wA_b[:, blk * 512:(blk + 1) * 512].rearrange("(c p) n -> p c n", p=128))
                        for ch in range(4):
                            slot = (pk % 3) * 2
                            pk += 1
                            pb = PS[slot]
                            for hf in range(nh):
                                for c in range(8):
                                    MM(P, pb, psum_t[:, slot + hf, :], w, w[:, c, ch * 128:(ch + 1) * 128], hT,
                                       hT[:, c, hf * 512:(hf + 1) * 512], c == 0, c == 7)
                            o = ob[ok % 4]
                            CP(P, 'act' if ok % 2 == 0 else 'dve', o, o[:, :].rearrange("p (h t) -> p h t", h=nh), pb,
                               psum_t[:, slot:slot + nh, :])
                            ok += 1
                            r0 = blk * 512 + ch * 128
                            P.store(o, fmT[r0:r0 + 128, t0:t0 + T], o[:])
                    w = wb[wk % 3]
                    wk += 1
                    P.load(w, w[:, :, 0:128], wA_b[:, 6656:6784].rearrange("(c p) n -> p c n", p=128))
                    slot = (pk % 3) * 2
                    pk += 1
                    pb = PS[slot]
                    for hf in range(nh):
                        for c in range(8):
                            MM(P, pb, psum_t[:, slot + hf, :], w, w[:, c, 0:128], hT, hT[:, c, hf * 512:(hf + 1) * 512], c == 0, c == 7)
                    CP(P, 'dve', osm, osm[:, :].rearrange("p (h t) -> p h t", h=nh), pb, psum_t[:, slot:slot + nh, :])
                    P.store(osm, smT[:, t0:t0 + T], osm[:])
                    for blk in range(3):
                        w = wb[wk % 3]
                        wk += 1
                        c0 = 6784 + blk * 512
                        P.load(w, w[:], wA_b[:, c0:c0 + 512].rearrange("(c p) n -> p c n", p=128))
                        for st in range(T // 128):
                            slot = (pk % 3) * 2
                            pk += 1
                            pb, pa = bank(slot)
                            for c in range(8):
                                MM(P, pb, pa, hT, hT[:, c, st * 128:(st + 1) * 128], w, w[:, c, :], c == 0, c == 7)
                            o = otm[ok % 3]
                            CP(P, 'act' if ok % 2 == 0 else 'dve', o, o[:], pb, pa)
                            ok += 1
                            P.store(o, tmv[t0 + st * 128:t0 + (st + 1) * 128, blk * 512:(blk + 1) * 512], o[:])
                P.barrier()

        def conv_fm(xp, acc, wcol, K, T, extra):
            TS(P, 'dve', acc, acc[:, :T], xp, xp[:, 0:T], wcol[:, 0:1], None, ALU.mult, extra_r=(wcol,))
            for j in range(1, K):
                STT(P, acc, acc[:, :T], xp, xp[:, j:j + T], wcol[:, j:j + 1], acc, acc[:, :T], ALU.mult, ALU.add, extra_r=(wcol,))

        def stage_mlstm(l):
            SEG = min(S, 2048)
            NCS = SEG // 128
            with contextlib.ExitStack() as es:
                tokQ = P.sb(es, "m_tokQ", [128, NC, 16], F32)
                decB = P.sb(es, "m_decB", [128, 4, NC], F32)
                with contextlib.ExitStack() as es2:
                    gi = P.sb(es2, "m_gi", [4, SEG], F32)
                    gf = P.sb(es2, "m_gf", [4, SEG], F32)
                    Gn = P.sb(es2, "m_Gn", [4, SEG], F32)
                    mt = P.sb(es2, "m_mt", [4, SEG], F32)
                    one4 = P.sb(es2, "m_one4", [4, SEG], F32)
                    tA = P.sb(es2, "m_tA", [4, SEG], F32)
                    Q1 = P.sb(es2, "m_Q1", [128, SEG], F32)
                    Q2 = P.sb(es2, "m_Q2", [128, SEG], F32)
                    mpe = P.sb(es2, "m_mpe", [4, NC + 1], F32)
                    dec = P.sb(es2, "m_dec", [4, NC], F32)
                    bi = P.sb(es2, "m_bi", [4, 1], F32)
                    bfn = P.sb(es2, "m_bfn", [4, 1], F32)
                    car = P.sb(es2, "m_car", [4, 2], F32)
                    P.load(bi, bi[:], mib_in[l])
                    P.load(bfn, bfn[:], mfb_in[l])
                    TS(P, 'dve', bfn, bfn[:], bfn, bfn[:], -1.0, None, ALU.mult)
                    MEMSET(P, 'dve', one4, one4[:], 1.0)
                    MEMSET(P, 'dve', Q1, Q1[:], 0.0)
                    MEMSET(P, 'dve', Q2, Q2[:], 0.0)
                    MEMSET(P, 'dve', mpe, mpe[:], 0.0)
                    MEMSET(P, 'dve', car, car[:], 0.0)
                    for sg in range(S // SEG):
                        t0 = sg * SEG
                        c0 = sg * NCS
                        P.load(gi, gi[:], smT[64:68, t0:t0 + SEG])
                        P.load(gf, gf[:], smT[68:72, t0:t0 + SEG])
                        ACT(P, gf, gf[:], gf, gf[:], AF.Exp, bias=bfn[:], scale=-1.0, extra_r=(bfn,))
                        ACT(P, gf, gf[:], gf, gf[:], AF.Ln, bias=onec[0:4, :], scale=1.0, extra_r=(onec,))
                        P.op('dve', lambda e: e.tensor_tensor_scan(out=Gn[:], data0=one4[:], data1=gf[:], initial=car[:, 0:1],
                                                                  op0=ALU.mult, op1=ALU.add), reads=(one4, gf, car), writes=(Gn,))
                        STT(P, gi, gi[:], gi, gi[:], bi[:, 0:1], Gn, Gn[:], ALU.add, ALU.add, extra_r=(bi,))
                        P.op('dve', lambda e: e.tensor_tensor_scan(out=mt[:], data0=one4[:], data1=gi[:], initial=car[:, 1:2],
                                                                  op0=ALU.mult, op1=ALU.max), reads=(one4, gi, car), writes=(mt,))
                        CP(P, 'dve', car, car[:, 0:1], Gn, Gn[:, SEG - 1:SEG])
                        CP(P, 'dve', car, car[:, 1:2], mt, mt[:, SEG - 1:SEG])
                        mt3 = mt[:].rearrange("p (c t) -> p c t", t=128)
                        CP(P, 'dve', mpe, mpe[:, c0 + 1:c0 + 1 + NCS], mt, mt3[:, :, 127])
                        mend_b = mpe[:, c0 + 1:c0 + 1 + NCS].unsqueeze(2).to_broadcast([4, NCS, 128])
                        mprev_b = mpe[:, c0:c0 + NCS].unsqueeze(2).to_broadcast([4, NCS, 128])
                        q3 = lambda b, p0: b[p0:p0 + 4, :].rearrange("p (c t) -> p c t", t=128)
                        tA3 = tA[:].rearrange("p (c t) -> p c t", t=128)
                        TT(P, 'dve', tA, tA3, mt, mt3, mpe, mend_b, ALU.subtract)
                        ACT(P, Q1, Q1[0:4, :], tA, tA[:], AF.Exp, scale=-1.0)
                        TT(P, 'dve', tA, tA3, mt, mt3, mpe, mprev_b, ALU.subtract)
                        ACT(P, Q1, Q1[32:36, :], tA, tA[:], AF.Exp, scale=-1.0)
                        TT(P, 'dve', tA, tA3, gi, gi[:].rearrange("p (c t) -> p c t", t=128), mpe, mend_b, ALU.subtract)
                        ACT(P, Q1, Q1[64:68, :], tA, tA[:], AF.Exp)
                        TT(P, 'dve', tA, tA[:], Gn, Gn[:], mt, mt[:], ALU.subtract)
                        ACT(P, Q2, Q2[0:4, :], tA, tA[:], AF.Exp)
                        for cg in range(NCS // 4):
                            pb, pa = bank(cg % 2)
                            pb2, pa2 = bank(2 + cg % 2)
                            for k in range(4):
                                c = cg * 4 + k
                                TR(P, pb, pa[:, k * 128:(k + 1) * 128], Q1, Q1[:, c * 128:(c + 1) * 128], idf, idf[:])
                                TR(P, pb2, pa2[:, k * 128:(k + 1) * 128], Q2, Q2[:, c * 128:(c + 1) * 128], idf, idf[:])
                            cc = c0 + cg * 4
                            CP(P, 'dve', tokQ, tokQ[:, cc:cc + 4, 0:12].rearrange("p c (j x) -> p c j x", j=3), pb,
                               pa.rearrange("p (c j x) -> p c j x", c=4, j=4)[:, :, 0:3, 0:4])
                            CP(P, 'act', tokQ, tokQ[:, cc:cc + 4, 12:16], pb2, pa2.rearrange("p (c x) -> p c x", c=4)[:, :, 0:4])
                    TT(P, 'dve', dec, dec[:], mpe, mpe[:, 0:NC], mpe, mpe[:, 1:NC + 1], ALU.subtract)
                    ACT(P, dec, dec[:], dec, dec[:], AF.Exp)
                    for h in range(4):
                        pb, pa = bank(4)
                        MM(P, pb, pa[:, 0:NC], selh, selh[:, h, :], dec, dec[:], True, True)
                        CP(P, 'dve', decB, decB[:, h, :], pb, pa[:, 0:NC])
                    P.barrier()
                TS(P, 'dve', tokQ, tokQ[:, :, 8:12], tokQ, tokQ[:, :, 8:12], 128.0 ** -0.5, None, ALU.mult)
                qT = P.sb(es, "m_qT", [128, S], BF16)
                kT = P.sb(es, "m_kT", [128, S], BF16)
                vx = P.sb(es, "m_vx", [128, NC, 129], BF16)
                Hall = P.sb(es, "m_H", [128, NC, 129], F32)
                scr = P.sb(es, "m_scr", [128, S], F32)
                yb = P.sb(es, "m_yb", [128, S + 8], BF16)
                wc = P.sb(es, "m_wc", [128, 4], F32)
                bc = P.sb(es, "m_bc", [128, 1], F32)
                gB = P.sb(es, "m_gB", [128, 128], F32)
                CTf = P.sb(es, "m_CTf", [128, 129], F32)
                CTb = P.sb(es, "m_CTb", [128, 129], BF16)
                Sm = [P.sb(es, f"m_Sm{i}", [128, 128], BF16) for i in range(2)]
                kp = [P.sb(es, f"m_kp{i}", [128, 128], BF16) for i in range(2)]
                st = P.sb(es, "m_st", [128, NC, 4], F32)
                scr3 = scr[:].rearrange("p (c d) -> p c d", d=128)
                for h in range(4):
                    for which, dstb in ((h, qT), (4 + h, kT)):
                        P.load(wc, wc[:], mcw_in[l, which])
                        P.load(bc, bc[:], mcb_in[l, which])
                        MEMSET(P, 'pool', yb, yb[:, 0:3], 0.0)
                        P.load(yb, yb[:, 3:3 + S], fmT[FM_QK + which * 128:FM_QK + (which + 1) * 128, :])
                        conv_fm(yb, scr, wc, 4, S, ())
                        ACT(P, dstb, dstb[:], scr, scr[:], AF.Silu, bias=bc[:], extra_r=(bc,))
                    MEMSET(P, 'pool', vx, vx[:, :, 128:129], 1.0)
                    P.load(vx, vx[:, :, 0:128], tmv[:, h * 128:(h + 1) * 128].rearrange("(c p) d -> p c d", p=128))
                    P.load(gB, gB[:], mng_in[l, h:h + 1, :].partition_broadcast(128))
                    MEMSET(P, 'dve', CTf, CTf[:], 0.0)
                    MEMSET(P, 'pool', CTb, CTb[:], 0.0)
                    for c in range(NC):
                        cs = slice(c * 128, (c + 1) * 128)
                        sm, kpp = Sm[c % 2], kp[c % 2]
                        b0, a0 = bank((c % 2) * 4 + 0)
                        b1, a1 = bank((c % 2) * 4 + 1)
                        b2, a2 = bank((c % 2) * 4 + 2)
                        b3, a3 = bank((c % 2) * 4 + 3)
                        MM(P, b0, a0[:, 0:128], kT, kT[:, cs], qT, qT[:, cs], True, True)
                        STT(P, sm, sm[:], b0, a0[:, 0:128], tokQ[:, c, 8 + h:9 + h], maskT, maskT[:], ALU.mult, ALU.mult, extra_r=(tokQ,))
                        MM(P, b1, a1[:, 0:129], sm, sm[:], vx, vx[:, c, :], True, True)
                        MM(P, b2, a2[:, 0:129], qT, qT[:, cs], CTb, CTb[:], True, True)
                        TS(P, 'dve', Hall, Hall[:, c, :], b1, a1[:, 0:129], tokQ[:, c, h:h + 1], None, ALU.mult, extra_r=(tokQ,))
                        STT(P, Hall, Hall[:, c, :], b2, a2[:, 0:129], tokQ[:, c, 4 + h:5 + h], Hall, Hall[:, c, :], ALU.mult, ALU.add, extra_r=(tokQ,))
                        kpb = psum_t[:, (c % 2) * 4 + 0, 256:384].bitcast(BF16)
                        TR(P, b0, kpb[:, 0:128], kT, kT[:, cs], idb, idb[:])
                        TS(P, 'dve', kpp, kpp[:], b0, kpb[:, 0:128], tokQ[:, c, 8 + h:9 + h], None, ALU.mult, extra_r=(tokQ,))
                        MM(P, b3, a3[:, 0:129], kpp, kpp[:], vx, vx[:, c, :], True, True)
                        STT(P, CTf, CTf[:], CTf, CTf[:], decB[:, h, c:c + 1], b3, a3[:, 0:129], ALU.mult, ALU.add, extra_r=(decB,))
                        CP(P, 'act', CTb, CTb[:], CTf, CTf[:])
                    den = Hall[:, :, 128]
                    STT(P, st, st[:, :, 0], Hall, den, -1.0, Hall, den, ALU.mult, ALU.max)
                    TT(P, 'dve', st, st[:, :, 0], st, st[:, :, 0], tokQ, tokQ[:, :, 12 + h], ALU.max)
                    P.op('dve', lambda e: e.reciprocal(st[:, :, 0], st[:, :, 0]), reads=(st,), writes=(st,))
                    H3 = Hall[:, :, 0:128]
                    TT(P, 'dve', Hall, H3, Hall, H3, st, st[:, :, 0:1].to_broadcast([128, NC, 128]), ALU.mult)
                    P.op('dve', lambda e: e.tensor_reduce(out=st[:, :, 1], in_=H3, axis=AX.X, op=ALU.add), reads=(Hall,), writes=(st,))
                    TS(P, 'dve', st, st[:, :, 1], st, st[:, :, 1], 1.0 / 128, None, ALU.mult)
                    TT(P, 'dve', Hall, H3, Hall, H3, st, st[:, :, 1:2].to_broadcast([128, NC, 128]), ALU.subtract)
                    TT(P, 'pool', scr, scr3, Hall, H3, Hall, H3, ALU.mult)
                    P.op('dve', lambda e: e.tensor_reduce(out=st[:, :, 2], in_=scr3, axis=AX.X, op=ALU.add), reads=(scr,), writes=(st,))
                    ACT(P, st, st[:, :, 2], st, st[:, :, 2], AF.Sqrt, bias=epsc[:], scale=1.0 / 128, extra_r=(epsc,))
                    P.op('dve', lambda e: e.reciprocal(st[:, :, 2], st[:, :, 2]), reads=(st,), writes=(st,))
                    TT(P, 'dve', Hall, H3, Hall, H3, st, st[:, :, 2:3].to_broadcast([128, NC, 128]), ALU.mult)
                    TT(P, 'dve', Hall, H3, Hall, H3, gB, gB[:].unsqueeze(1).to_broadcast([128, NC, 128]), ALU.mult)
                    kT3 = kT[:].rearrange("p (c d) -> p c d", d=128)
                    P.load(kT, kT3, tmv[:, 512 + h * 128:512 + (h + 1) * 128].rearrange("(c p) d -> p c d", p=128))
                    ACT(P, scr, scr3, kT, kT3, AF.Sigmoid)
                    yb3 = yb[:, 0:S].rearrange("p (c d) -> p c d", d=128)
                    TT(P, 'dve', yb, yb3, Hall, H3, scr, scr3, ALU.mult)
                    for cg in range(NC // 4):
                        pbk = PS[cg % 2]
                        pv = psum_t[:, cg % 2, 0:256].bitcast(BF16)
                        for k in range(4):
                            c = cg * 4 + k
                            TR(P, pbk, pv[:, k * 128:(k + 1) * 128], yb, yb[:, c * 128:(c + 1) * 128], idb, idb[:])
                        CP(P, 'act' if cg % 2 else 'dve', qT, qT[:, cg * 512:(cg + 1) * 512], pbk, pv[:, 0:512])
                    P.store(qT, yT[h * 128:(h + 1) * 128, :], qT[:])
                P.barrier()

        def stage_rglru(l):
            SEG = min(S, 2048)
            with contextlib.ExitStack() as es:
                wab = P.sb(es, "r_wab", [128, 128], BF16)
                wxb = P.sb(es, "r_wxb", [128, 128], BF16)
                wtmp = P.sb(es, "r_wtmp", [128, 128], F32)
                wc = P.sb(es, "r_wc", [128, 4], F32)
                cols = P.sb(es, "r_cols", [128, 8], F32)
                xp = P.sb(es, "r_xp", [128, SEG + 3], BF16)
                xc = P.sb(es, "r_xc", [128, SEG], F32)
                xcb = P.sb(es, "r_xcb", [128, SEG], BF16)
                rr = P.sb(es, "r_rr", [128, SEG], F32)
                ii = P.sb(es, "r_ii", [128, SEG], F32)
                aa = P.sb(es, "r_aa", [128, SEG], F32)
                hh = P.sb(es, "r_hh", [128, SEG], F32)
                rg = P.sb(es, "r_rg", [128, SEG], BF16)
                yo = P.sb(es, "r_yo", [128, SEG], BF16)
                car = P.sb(es, "r_car", [128, 1], F32)
                for ch in range(4):
                    P.load(wtmp, wtmp[:], rwa_in[l, ch])
                    CP(P, 'dve', wab, wab[:], wtmp, wtmp[:])
                    P.load(wtmp, wtmp[:], rwx_in[l, ch])
                    CP(P, 'dve', wxb, wxb[:], wtmp, wtmp[:])
                    P.load(wc, wc[:], rcw_in[l, ch])
                    P.load(cols, cols[:, 0:1], rcb_in[l, ch])
                    P.load(cols, cols[:, 1:2], rba_in[l, ch])
                    P.load(cols, cols[:, 2:3], rbx_in[l, ch])
                    P.load(cols, cols[:, 3:4], rlam_in[l, ch])
                    ACT(P, cols, cols[:, 3:4], cols, cols[:, 3:4], AF.Exp, scale=-1.0)
                    ACT(P, cols, cols[:, 3:4], cols, cols[:, 3:4], AF.Ln, bias=onec[:], scale=1.0, extra_r=(onec,))
                    TS(P, 'dve', cols, cols[:, 4:5], cols, cols[:, 3:4], -16.0, None, ALU.mult)
                    TS(P, 'dve', cols, cols[:, 3:4], cols, cols[:, 3:4], -8.0, None, ALU.mult)
                    MEMSET(P, 'dve', car, car[:], 0.0)
                    for sg in range(S // SEG):
                        t0 = sg * SEG
                        r0 = FM_RX + ch * 128
                        if sg == 0:
                            MEMSET(P, 'pool', xp, xp[:, 0:3], 0.0)
                            P.load(xp, xp[:, 3:3 + SEG], fmT[r0:r0 + 128, 0:SEG])
                        else:
                            P.load(xp, xp[:, 0:3 + SEG], fmT[r0:r0 + 128, t0 - 3:t0 + SEG])
                        P.load(rg, rg[:], fmT[FM_RG + ch * 128:FM_RG + (ch + 1) * 128, t0:t0 + SEG])
                        conv_fm(xp, xc, wc, 4, SEG, ())
                        TS(P, 'dve', xc, xc[:], xc, xc[:], cols[:, 0:1], None, ALU.add, extra_r=(cols,))
                        CP(P, 'act', xcb, xcb[:], xc, xc[:])
                        for q in range(SEG // 512):
                            qs = slice(q * 512, (q + 1) * 512)
                            pb, pa = bank(q % 4)
                            MM(P, pb, pa, wab, wab[:], xcb, xcb[:, qs], True, True)
                            ACT(P, rr, rr[:, qs], pb, pa, AF.Sigmoid, bias=cols[:, 1:2], extra_r=(cols,))
                            pb, pa = bank(4 + q % 4)
                            MM(P, pb, pa, wxb, wxb[:], xcb, xcb[:, qs], True, True)
                            ACT(P, ii, ii[:, qs], pb, pa, AF.Sigmoid, bias=cols[:, 2:3], extra_r=(cols,))
                        ACT(P, aa, aa[:], rr, rr[:], AF.Exp, scale=cols[:, 3:4], extra_r=(cols,))
                        ACT(P, rr, rr[:], rr, rr[:], AF.Exp, scale=cols[:, 4:5], extra_r=(cols,))
                        TS(P, 'dve', rr, rr[:], rr, rr[:], -1.0, 1.0, ALU.mult, ALU.add)
                        ACT(P, rr, rr[:], rr, rr[:], AF.Sqrt)
                        TT(P, 'dve', ii, ii[:], ii, ii[:], xc, xc[:], ALU.mult)
                        TT(P, 'dve', ii, ii[:], ii, ii[:], rr, rr[:], ALU.mult)
                        P.op('dve', lambda e: e.tensor_tensor_scan(out=hh[:], data0=aa[:], data1=ii[:], initial=car[:, 0:1],
                                                                  op0=ALU.mult, op1=ALU.add), reads=(aa, ii, car), writes=(hh,))
                        CP(P, 'dve', car, car[:], hh, hh[:, SEG - 1:SEG])
                        ACT(P, xc, xc[:], rg, rg[:], AF.Gelu_apprx_tanh)
                        TT(P, 'dve', yo, yo[:], xc, xc[:], hh, hh[:], ALU.mult)
                        P.store(yo, yT[512 + ch * 128:512 + (ch + 1) * 128, t0:t0 + SEG], yo[:])
                P.barrier()

        def stage_rope_tables():
            T = min(S, 2048)
            C1 = 6.28125
            C2 = float(np.float32(round((2 * math.pi - C1) * 2 ** 20) / 2 ** 20))
            C3 = float(2 * math.pi - C1 - C2)
            MAGIC = 12582912.0
            with contextlib.ExitStack() as es:
                pi_ = P.sb(es, "t_pi", [128, T], I32)
                ang = P.sb(es, "t_ang", [128, T], F32)
                kf = P.sb(es, "t_kf", [128, T], F32)
                sn = P.sb(es, "t_sn", [128, T], F32)
                cs = P.sb(es, "t_cs", [128, T], F32)
                for t0 in range(0, S, T):
                    P.load(pi_, pi_[:], pos_in[0:1, t0:t0 + T].partition_broadcast(128))
                    CP(P, 'dve', ang, ang[:], pi_, pi_[:])
                    TS(P, 'dve', ang, ang[:], ang, ang[:], invf[:, 0:1], None, ALU.mult, extra_r=(invf,))
                    TS(P, 'dve', kf, kf[:], ang, ang[:], 1.0 / (2 * math.pi), MAGIC, ALU.mult, ALU.add)
                    TS(P, 'dve', kf, kf[:], kf, kf[:], MAGIC, None, ALU.subtract)
                    for cc in (C1, C2, C3):
                        STT(P, ang, ang[:], kf, kf[:], -cc, ang, ang[:], ALU.mult, ALU.add)
                    TS(P, 'dve', ang, ang[:], ang, ang[:], 3.1415925, -3.1415925, ALU.min, ALU.max)
                    ACT(P, sn, sn[:], ang, ang[:], AF.Sin)
                    ACT(P, cs, cs[:], ang, ang[:], AF.Sin, scale=0.5)
                    TT(P, 'dve', cs, cs[:], cs, cs[:], cs, cs[:], ALU.mult)
                    TS(P, 'dve', cs, cs[:], cs, cs[:], -2.0, 1.0, ALU.mult, ALU.add)
                    P.store(sn, sinT[:, t0:t0 + T], sn[:])
                    P.store(cs, cosT[:, t0:t0 + T], cs[:])
                P.barrier()

        def stage_rope(l):
            T = 512
            with contextlib.ExitStack() as es:
                cs = [P.sb(es, f"p_cs{i}", [128, T], F32) for i in range(2)]
                sn = [P.sb(es, f"p_sn{i}", [128, T], F32) for i in range(2)]
                a = [P.sb(es, f"p_a{i}", [128, T], BF16) for i in range(3)]
                t1 = [P.sb(es, f"p_t1{i}", [128, T], F32) for i in range(2)]
                t2 = [P.sb(es, f"p_t2{i}", [128, T], F32) for i in range(2)]
                o = [P.sb(es, f"p_o{i}", [128, T], BF16) for i in range(3)]
                sm = [P.sb(es, f"p_sm{i}", [128, T], F32) for i in range(2)]
                w8 = [P.sb(es, f"p_w8{i}", [128, T], F32) for i in range(2)]
                w8a = [P.sb(es, f"p_w8a{i}", [8, T], F32) for i in range(2)]
                for i in range(2):
                    MEMSET(P, 'dve', w8[i], w8[i][:], 0.0)
                kib = P.sb(es, "p_kib", [128, T], BF16)
                sg = [P.sb(es, f"p_sg{i}", [128, 4, 8], F32) for i in range(2)]
                vt = [P.sb(es, f"p_vt{i}", [128, 512], BF16) for i in range(2)]
                vx = [P.sb(es, f"p_vx{i}", [128, 8, 128], BF16) for i in range(2)]
                for i in range(2):
                    MEMSET(P, 'pool', vx[i], vx[i][:], 1.0)
                k = 0
                for ti, t0 in enumerate(range(0, S, T)):
                    c_, s_ = cs[ti % 2], sn[ti % 2]
                    P.load(c_, c_[:], cosT[:, t0:t0 + T])
                    P.load(s_, s_[:], sinT[:, t0:t0 + T])
                    w = w8[ti % 2]
                    wa = w8a[ti % 2]
                    P.load(w, w[0:8, :], smT[72:80, t0:t0 + T])
                    STT(P, wa, wa[:], w, w[0:8, :], -1.0, w, w[0:8, :], ALU.mult, ALU.max)
                    sgb = sg[ti % 2]
                    pb, pa = bank(7)
                    for j in range(4):
                        TR(P, pb, pa[:, j * 128:(j + 1) * 128], w, w[:, j * 128:(j + 1) * 128], idf, idf[:])
                    ACT(P, sgb, sgb[:], pb, pa.rearrange("p (j x) -> p j x", j=4)[:, :, 0:8], AF.Sign)
                    P.store(sgb, sgn_d[t0:t0 + T, :].rearrange("(j p) h -> p j h", p=128), sgb[:])

                    def rope_chunk(src_b, src_ap, scale_bank=None):
                        nonlocal k
                        t1b, t2b, ob = t1[k % 2], t2[k % 2], o[k % 3]
                        pb, pa = bank(k % 4)
                        MM(P, pb, pa, rot, rot[:], src_b, src_ap, True, True)
                        TT(P, 'dve', t1b, t1b[:], src_b, src_ap, c_, c_[:], ALU.mult)
                        TT(P, 'dve', t2b, t2b[:], pb, pa, s_, s_[:], ALU.mult)
                        if scale_bank is None:
                            TT(P, 'dve', ob, ob[:], t1b, t1b[:], t2b, t2b[:], ALU.add)
                        else:
                            TT(P, 'pool', t1b, t1b[:], t1b, t1b[:], t2b, t2b[:], ALU.add)
                            TT(P, 'dve', ob, ob[:], scale_bank[0], scale_bank[1], t1b, t1b[:], ALU.mult)
                        k += 1
                        return ob

                    for ch in range(4):
                        ab = a[k % 3]
                        P.load(ab, ab[:], fmT[FM_AQ + ch * 128:FM_AQ + (ch + 1) * 128, t0:t0 + T])
                        ob = rope_chunk(ab, ab[:])
                        P.store(ob, qrT[ch * 128:(ch + 1) * 128, t0:t0 + T], ob[:])
                    for ch in range(4):
                        ab = a[k % 3]
                        P.load(ab, ab[:], fmT[FM_AK + ch * 128:FM_AK + (ch + 1) * 128, t0:t0 + T])
                        ob = rope_chunk(ab, ab[:])
                        P.store(ob, kblk[t0 // 128:t0 // 128 + 4, :, ch, :].rearrange("j p s -> p j s"),
                                ob[:].rearrange("p (j s) -> p j s", j=4))
                    for ch in range(4):
                        ab = a[k % 3]
                        P.load(ab, ab[:], fmT[FM_QI + ch * 128:FM_QI + (ch + 1) * 128, t0:t0 + T])
                        pbw, paw = bank(4 + ch % 2)
                        MM(P, pbw, paw, sel8, sel8[:, ch, :], wa, wa[:], True, True)
                        ob = rope_chunk(ab, ab[:], scale_bank=(pbw, paw))
                        P.store(ob, qiT[ch * 128:(ch + 1) * 128, t0:t0 + T], ob[:])
                    smb = sm[ti % 2]
                    P.load(smb, smb[0:64, :], smT[0:64, t0:t0 + T])
                    MEMSET(P, 'pool', kib, kib[:], 0.0)
                    CP(P, 'act', kib, kib[0:64, :], smb, smb[0:64, :])
                    ob = rope_chunk(kib, kib[:])
                    P.store(ob, kiT[:, t0:t0 + T], ob[0:64, :])
                    for j in range(4):
                        vtb, vxb = vt[j % 2], vx[j % 2]
                        tt = t0 + j * 128
                        P.load(vtb, vtb[:], tmv[tt:tt + 128, 1024:1536])
                        v4 = vtb[:].rearrange("p (h two d) -> p h two d", two=2, d=64)
                        x4 = vxb[:].rearrange("p (h two) d -> p h two d", two=2)
                        CP(P, 'dve', vxb, x4[:, :, 0, 0:64], vtb, v4[:, :, 0, :])
                        CP(P, 'dve', vxb, x4[:, :, 1, 64:128], vtb, v4[:, :, 1, :])
                        P.store(vxb, vblk[tt // 128], vxb[:])
                P.barrier()

        def stage_dsa(l):
            with contextlib.ExitStack() as es:
                ki2 = P.sb(es, "d_ki2", [128, S], BF16)
                Irow = P.sb(es, "d_Irow", [128, S], F32)
                junk = P.sb(es, "d_junk", [128, S], BF16)
                mrow = P.sb(es, "d_mrow", [128, S], BF16)
                junk2 = P.sb(es, "d_junk2", [128, S // 2 + 128], BF16)
                mT = [P.sb(es, f"d_mT{i}", [128, NC, 128], BF16) for i in range(2)]
                rl = [P.sb(es, f"d_rl{i}", [128, 2, 512], BF16) for i in range(3)]
                Eb = [P.sb(es, f"d_E{i}", [128, 8, 128], BF16) for i in range(3)]
                kb = [P.sb(es, f"d_kb{i}", [128, 4, 128], BF16) for i in range(3)]
                vb = [P.sb(es, f"d_vb{i}", [128, 8, 128], BF16) for i in range(3)]
                qi_t = [P.sb(es, f"d_qi{i}", [128, 4, 128], BF16) for i in range(2)]
                q_t = [P.sb(es, f"d_q{i}", [128, 4, 128], BF16) for i in range(2)]
                sg_t = [P.sb(es, f"d_sg{i}", [128, 8], F32) for i in range(2)]
                Dall = [P.sb(es, f"d_D{i}", [128, 8, 128], BF16) for i in range(2)]
                bs = [P.sb(es, f"d_bs{i}", [128, 24], F32) for i in range(2)]
                bsa = [P.sb(es, f"d_bsa{i}", [128, 2], F32) for i in range(2)]
                osb = [P.sb(es, f"d_os{i}", [128, 8, 128], F32) for i in range(2)]
                rcp = [P.sb(es, f"d_rc{i}", [128, 8, 128], F32) for i in range(2)]
                yc = [P.sb(es, f"d_yc{i}", [128, 4, 128], BF16) for i in range(2)]
                P.load(ki2, ki2[0:64, :], kiT[:, :])
                P.load(ki2, ki2[64:128, :], kiT[:, :])
                P.barrier()
                ek = 0
                lk = 0
                for ti in range(NC):
                    t0 = ti * 128
                    n = t0 + 128
                    qi_, q_, sg_, D_, b_ = qi_t[ti % 2], q_t[ti % 2], sg_t[ti % 2], Dall[ti % 2], bs[ti % 2]
                    P.load(qi_, qi_[:], qiT[:, t0:t0 + 128].rearrange("(c p) t -> p c t", p=128))
                    P.load(q_, q_[:], qrT[:, t0:t0 + 128].rearrange("(c p) t -> p c t", p=128))
                    P.load(sg_, sg_[:], sgn_d[t0:t0 + 128, :])
                    for h in range(8):
                        TS(P, 'pool' if h % 2 else 'dve', D_, D_[:, h, :], idb, idb[:], sg_[:, h:h + 1], None, ALU.mult, extra_r=(sg_,))
                    for s0 in range(0, n, 512):
                        wdt = min(512, n - s0)
                        sb_, sa_ = bank(4 + (s0 // 512) % 2)
                        for hp in range(4):
                            r_ = rl[lk % 3]
                            slot = (lk % 2) * 2
                            lb = PS[slot]
                            lk += 1
                            for e2 in range(2):
                                h = hp * 2 + e2
                                p0 = (h % 2) * 64
                                MM(P, lb, psum_t[:, slot + e2, 0:wdt], qi_, qi_[p0:p0 + 64, h // 2, :], ki2,
                                   ki2[p0:p0 + 64, s0:s0 + wdt], True, True)
                            ACT(P, r_, r_[:, :, 0:wdt], lb, psum_t[:, slot:slot + 2, 0:wdt], AF.Relu)
                            for e2 in range(2):
                                h = hp * 2 + e2
                                MM(P, sb_, sa_[:, 0:wdt], D_, D_[:, h, :], r_, r_[:, e2, 0:wdt], h == 0, h == 7)
                        CP(P, 'dve', Irow, Irow[:, s0:s0 + wdt], sb_, sa_[:, 0:wdt])
                    P.op('dve', lambda e: e.tensor_reduce(out=b_[:, 0:1], in_=Irow[:, 0:n], axis=AX.X, op=ALU.min), reads=(Irow,), writes=(b_,))
                    P.op('dve', lambda e: e.tensor_reduce(out=b_[:, 1:2], in_=Irow[:, 0:n], axis=AX.X, op=ALU.max), reads=(Irow,), writes=(b_,))
                    TT(P, 'dve', Irow, Irow[:, t0:n], Irow, Irow[:, t0:n], negm, negm[:], ALU.add)
                    TS(P, 'dve', b_, b_[:, 0:1], b_, b_[:, 0:1], -1.0, None, ALU.add)
                    TT(P, 'dve', b_, b_[:, 2:3], b_, b_[:, 1:2], b_, b_[:, 0:1], ALU.subtract)
                    TS(P, 'dve', b_, b_[:, 8:8 + NBIS + 1], cvec, cvec[:, 0:NBIS + 1], b_[:, 2:3], None, ALU.mult, extra_r=(b_,))
                    TT(P, 'dve', b_, b_[:, 3:4], b_, b_[:, 0:1], b_, b_[:, 8:9], ALU.add)
                    h0 = ((n // 2 + 127) // 128) * 128 if n > 128 else n
                    nB = n - h0
                    kk = min(256, S // 4) - 0.5
                    ba_ = bsa[ti % 2]
                    for it in range(NBIS):
                        if nB > 0:
                            ACT(P, junk2, junk2[:, 0:nB], Irow, Irow[:, h0:n], AF.Sign, bias=b_[:, 3:4], scale=-1.0,
                                accum=ba_[:, 0:1], extra_r=(b_,), extra_w=(ba_,))
                        TS(P, 'dve', junk, junk[:, 0:h0], Irow, Irow[:, 0:h0], b_[:, 3:4], None, ALU.is_ge, ALU.add,
                           accum=b_[:, 4:5], extra_r=(b_,), extra_w=(b_,))
                        if nB > 0:
                            STT(P, b_, b_[:, 4:5], ba_, ba_[:, 0:1], -0.5, b_, b_[:, 4:5], ALU.mult, ALU.add)
                        TS(P, 'dve', b_, b_[:, 5:6], b_, b_[:, 4:5], kk - nB / 2.0, 0.5, ALU.is_ge, ALU.subtract)
                        STT(P, b_, b_[:, 3:4], b_, b_[:, 5:6], b_[:, 8 + it:9 + it], b_, b_[:, 3:4], ALU.mult, ALU.add)
                    STT(P, b_, b_[:, 3:4], b_, b_[:, 8 + NBIS - 1:9 + NBIS - 1], -1.0001, b_, b_[:, 3:4], ALU.mult, ALU.add)
                    TS(P, 'dve', mrow, mrow[:, 0:n], Irow, Irow[:, 0:n], b_[:, 3:4], 1.0, ALU.is_ge, ALU.subtract, extra_r=(b_,))
                    mT_ = mT[ti % 2]
                    for jg in range(0, ti + 1, 8):
                        nj = min(8, ti + 1 - jg)
                        pbk = PS[6 + (jg // 8) % 2]
                        pv = psum_t[:, 6 + (jg // 8) % 2, :].bitcast(BF16)
                        for jj in range(nj):
                            j = jg + jj
                            TR(P, pbk, pv[:, jj * 128:(jj + 1) * 128], mrow, mrow[:, j * 128:(j + 1) * 128], idb, idb[:])
                        CP(P, 'act', mT_, mT_[:, jg:jg + nj, :], pbk, pv[:, 0:nj * 128].rearrange("p (j t) -> p j t", j=nj))
                    for j in range(ti + 1):
                        kb_, vb_ = kb[j % 3], vb[j % 3]
                        P.load(kb_, kb_[:], kblk[j])
                        P.load(vb_, vb_[:], vblk[j])
                        e_ = Eb[ek % 3]
                        ek += 1
                        qb = PS[0]
                        for bk in range(2):
                            for x in range(4):
                                MM(P, qb, psum_t[:, bk, x * 128:(x + 1) * 128], id240, id240[:], mT_, mT_[:, j, :], x == 0, False)
                            for x in range(4):
                                h = 2 * x + bk
                                p0 = bk * 64
                                MM(P, qb, psum_t[:, bk, x * 128:(x + 1) * 128], kb_, kb_[p0:p0 + 64, x, :], q_,
                                   q_[p0:p0 + 64, x, :], False, True)
                        ACT(P, e_, e_[:], qb, psum_t[:, 0:2, :].rearrange("p b (h t) -> p (b h) t", h=4), AF.Exp, scale=0.125)
                        ob_ = PS[2]
                        for h in range(8):
                            MM(P, ob_, psum_t[:, 2 + h // 4, (h % 4) * 128:(h % 4 + 1) * 128], vb_, vb_[:, h, :], e_,
                               e_[:, (h % 2) * 4 + h // 2, :], j == 0 and h % 4 == 0, j == ti)
                    os_, rc_, yc_ = osb[ti % 2], rcp[ti % 2], yc[ti % 2]
                    CP(P, 'dve', os_, os_[:], PS[2], psum_t[:, 2:4, :].rearrange("p b (h t) -> p (b h) t", h=4))
                    o4 = os_[:].rearrange("p (c two) t -> p c two t", two=2)
                    r4 = rc_[:].rearrange("p (c two) t -> p c two t", two=2)
                    ACT(P, rc_, r4[0:64, :, 0, :], os_, o4[64:128, :, 0, :], AF.Copy)
                    ACT(P, rc_, r4[64:128, :, 1, :], os_, o4[0:64, :, 1, :], AF.Copy)
                    P.op('dve', lambda e: e.reciprocal(r4[0:64, :, 0, :], r4[0:64, :, 0, :]), reads=(rc_,), writes=(rc_,))
                    P.op('dve', lambda e: e.reciprocal(r4[64:128, :, 1, :], r4[64:128, :, 1, :]), reads=(rc_,), writes=(rc_,))
                    TT(P, 'dve', yc_, yc_[0:64, :, :], os_, o4[0:64, :, 0, :], rc_, r4[0:64, :, 0, :], ALU.mult)
                    TT(P, 'dve', yc_, yc_[64:128, :, :], os_, o4[64:128, :, 1, :], rc_, r4[64:128, :, 1, :], ALU.mult)
                    P.store(yc_, yT[1024:1536, t0:t0 + 128].rearrange("(c p) t -> p c t", p=128), yc_[:])
                P.barrier()

        def stage_merge(l):
            T = 512
            with contextlib.ExitStack() as es:
                wbr = P.sb(es, "e_wbr", [128, 12, D], BF16)
                wo = P.sb(es, "e_wo", [128, 8, D], BF16)
                P.load(wbr, wbr[:], wbr_b.rearrange("(c p) n -> p c n", p=128))
                P.load(wo, wo[:], wo_b.rearrange("(c p) n -> p c n", p=128))
                yt = [P.sb(es, f"e_y{i}", [128, 12, T], BF16) for i in range(2)]
                gt = [P.sb(es, f"e_g{i}", [128, 24, T], BF16) for i in range(2)]
                xt = [P.sb(es, f"e_x{i}", [128, 8, T], F32) for i in range(2)]
                mg = [P.sb(es, f"e_m{i}", [128, 8, T], BF16) for i in range(2)]
                acc = [P.sb(es, f"e_a{i}", [128, T], F32) for i in range(2)]
                tmp = [P.sb(es, f"e_t{i}", [128, T], F32) for i in range(2)]
                k = 0
                for ti, t0 in enumerate(range(0, S, T)):
                    y_, g_, x_, m_ = yt[ti % 2], gt[ti % 2], xt[ti % 2], mg[ti % 2]
                    P.load(y_, y_[:], yT[:, t0:t0 + T].rearrange("(c p) t -> p c t", p=128))
                    P.load(g_, g_[:], fmT[FM_G:FM_G + 3072, t0:t0 + T].rearrange("(c p) t -> p c t", p=128))
                    P.load(x_, x_[:], xT[:, t0:t0 + T].rearrange("(c p) t -> p c t", p=128))
                    ACT(P, g_, g_[:], g_, g_[:], AF.Sigmoid)
                    for nn in range(8):
                        a_, t_ = acc[nn % 2], tmp[nn % 2]
                        for br in range(3):
                            pb, pa = bank((k % 2) * 3 + br)
                            for c in range(4):
                                MM(P, pb, pa, wbr, wbr[:, br * 4 + c, nn * 128:(nn + 1) * 128], y_, y_[:, br * 4 + c, :], c == 0, c == 3)
                            if br == 0:
                                TT(P, 'dve', a_, a_[:], pb, pa, g_, g_[:, nn, :], ALU.mult)
                            else:
                                TT(P, 'dve', t_, t_[:], pb, pa, g_, g_[:, br * 8 + nn, :], ALU.mult)
                                if br == 1:
                                    TT(P, 'pool', a_, a_[:], a_, a_[:], t_, t_[:], ALU.add)
                                else:
                                    TT(P, 'dve', m_, m_[:, nn, :], a_, a_[:], t_, t_[:], ALU.add)
                        k += 1
                    for nn in range(8):
                        pb, pa = bank(6 + nn % 2)
                        for c in range(8):
                            MM(P, pb, pa, wo, wo[:, c, nn * 128:(nn + 1) * 128], m_, m_[:, c, :], c == 0, c == 7)
                        TT(P, 'dve', x_, x_[:, nn, :], x_, x_[:, nn, :], pb, pa, ALU.add)
                    P.store(x_, xT[:, t0:t0 + T].rearrange("(c p) t -> p c t", p=128), x_[:])
                P.barrier()

        def stage_ffn(l):
            T = 512
            with contextlib.ExitStack() as es:
                nb = (P.sb(es, "f_xt", [128, 8, T], F32), P.sb(es, "f_sq", [128, 8, T], BF16),
                      P.sb(es, "f_hT", [128, 8, T], BF16), P.sb(es, "f_rstd", [128, T], F32))
                wu = [P.sb(es, f"f_wu{i}", [128, 8, 512], BF16) for i in range(3)]
                wd = [P.sb(es, f"f_wd{i}", [128, 24, 256], BF16) for i in range(2)]
                cw = P.sb(es, "f_cw", [128, 24, 3], F32)
                cb = P.sb(es, "f_cb", [128, 24], F32)
                G = [P.sb(es, f"f_G{i}", [128, T + 2], F32) for i in range(3)]
                carry = P.sb(es, "f_carry", [128, 24, 2], F32)
                accs = [P.sb(es, f"f_acc{i}", [128, T], F32) for i in range(3)]
                at = P.sb(es, "f_at", [128, 24, T], BF16)
                P.load(cw, cw[:], fcw_in[l])
                P.load(cb, cb[:], fcb_in[l])
                MEMSET(P, 'dve', carry, carry[:], 0.0)
                wk = 0
                gk = 0
                for t0 in range(0, S, T):
                    xt, hT = rmsnorm_tile(nb, t0, T)
                    for blk in range(6):
                        wg_, wu_ = wu[wk % 3], wu[(wk + 1) % 3]
                        wk += 2
                        P.load(wg_, wg_[:], fup_b[:, blk * 512:(blk + 1) * 512].rearrange("(c p) n -> p c n", p=128))
                        P.load(wu_, wu_[:], fup_b[:, DFF + blk * 512:DFF + (blk + 1) * 512].rearrange("(c p) n -> p c n", p=128))
                        for ch in range(4):
                            f = blk * 4 + ch
                            pbg, pag = bank((gk % 3) * 2)
                            pbu, pau = bank((gk % 3) * 2 + 1)
                            for c in range(8):
                                MM(P, pbg, pag, wg_, wg_[:, c, ch * 128:(ch + 1) * 128], hT, hT[:, c, :], c == 0, c == 7)
                            for c in range(8):
                                MM(P, pbu, pau, wu_, wu_[:, c, ch * 128:(ch + 1) * 128], hT, hT[:, c, :], c == 0, c == 7)
                            g_, a_ = G[gk % 3], accs[gk % 3]
                            gk += 1
                            CP(P, 'dve', g_, g_[:, 0:2], carry, carry[:, f, :])
                            CP(P, 'act', g_, g_[:, 2:2 + T], pbg, pag)
                            CP(P, 'dve', carry, carry[:, f, :], g_, g_[:, T:T + 2])
                            TS(P, 'dve', a_, a_[:], g_, g_[:, 0:T], cw[:, f, 0:1], None, ALU.mult, extra_r=(cw,))
                            STT(P, a_, a_[:], g_, g_[:, 1:T + 1], cw[:, f, 1:2], a_, a_[:], ALU.mult, ALU.add, extra_r=(cw,))
                            STT(P, a_, a_[:], g_, g_[:, 2:T + 2], cw[:, f, 2:3], a_, a_[:], ALU.mult, ALU.add, extra_r=(cw,))
                            ACT(P, a_, a_[:], a_, a_[:], AF.Gelu_apprx_tanh, bias=cb[:, f:f + 1], extra_r=(cb,))
                            TT(P, 'dve', at, at[:, f, :], a_, a_[:], pbu, pau, ALU.mult)
                    for half in range(4):
                        w_ = wd[half % 2]
                        P.load(w_, w_[:], fdn_b[:, half * 256:(half + 1) * 256].rearrange("(c p) n -> p c n", p=128))
                        for n2 in range(2):
                            nn = half * 2 + n2
                            pb, pa = bank(6 + nn % 2)
                            for c in range(24):
                                MM(P, pb, pa, w_, w_[:, c, n2 * 128:(n2 + 1) * 128], at, at[:, c, :], c == 0, c == 23)
                            TT(P, 'dve', xt, xt[:, nn, :T], xt, xt[:, nn, :T], pb, pa, ALU.add)
                    P.store(xt, xT[:, t0:t0 + T].rearrange("(c p) t -> p c t", p=128), xt[:, :, :T])
                P.barrier()

        def stage_final():
            T = 512
            with contextlib.ExitStack() as es:
                nb = (P.sb(es, "z_xt", [128, 8, T], F32), P.sb(es, "z_sq", [128, 8, T], BF16),
                      P.sb(es, "z_hT", [128, 8, T], F32), P.sb(es, "z_rstd", [128, T], F32))
                gc = P.sb(es, "z_g", [128, 8], F32)
                P.load(gc, gc[:], gfin_in)
                ob = [P.sb(es, f"z_o{i}", [128, D], F32) for i in range(2)]
                k = 0
                for t0 in range(0, S, T):
                    xt, hT = rmsnorm_tile(nb, t0, T, gcol=gc)
                    for st in range(4):
                        o = ob[k % 2]
                        for hlf in range(2):
                            pb, pa = bank((k % 2) * 2 + hlf)
                            for c in range(4):
                                cc = hlf * 4 + c
                                TR(P, pb, pa[:, c * 128:(c + 1) * 128], hT, hT[:, cc, st * 128:(st + 1) * 128], idf, idf[:])
                            CP(P, 'dve' if hlf == 0 else 'act', o, o[:, hlf * 512:(hlf + 1) * 512], pb, pa)
                        k += 1
                        P.store(o, out_d[t0 + st * 128:t0 + (st + 1) * 128, :], o[:])
                P.barrier()

        stage_transpose_in()
        if stop >= 1:
            stage_rope_tables()
        for l in range(depth):
            if stop >= 2:
                convert("wA", wA_in[l], wA_b, D, NA, gmix_in[l])
                convert("wbr", wbr_in[l], wbr_b, 1536, D)
                convert("wo", wo_in[l], wo_b, D, D)
                convert("fup", fup_in[l], fup_b, D, 2 * DFF, gffn_in[l])
                convert("fdn", fdn_in[l], fdn_b, DFF, D)
            if stop >= 3:
                stage_inproj()
            if stop >= 4:
                stage_mlstm(l)
            if stop >= 5:
                stage_rglru(l)
            if stop >= 6:
                stage_rope(l)
            if stop >= 7:
                stage_dsa(l)
            if stop >= 8:
                stage_merge(l)
            if stop >= 9:
                stage_ffn(l)
        if stop >= 10:
            stage_final()
    print("ninst", P.ninst, "nwait", P.nwait, "pool", len(P.pool))
    return nc


def make_consts():
    bf = ml_dtypes.bfloat16
    idf = np.eye(128, dtype=np.float32)
    s = np.arange(128)
    maskT = (s[:, None] <= s[None, :]).astype(np.float32)
    negm = np.where(s[None, :] <= s[:, None], 0.0, -1e30).astype(np.float32)
    rot = np.zeros((128, 128), np.float32)
    for m in range(128):
        hm, dm = divmod(m, 64)
        if dm < 32:
            rot[hm * 64 + dm + 32, m] = -1.0
        else:
            rot[hm * 64 + dm - 32, m] = 1.0
    sel8 = np.zeros((8, 4, 128), np.float32)
    for ch in range(4):
        for m in range(128):
            sel8[2 * ch + (m >= 64), ch, m] = 1.0
    selh = np.zeros((4, 4, 128), np.float32)
    for h in range(4):
        selh[h, h, :] = 1.0
    cvec = np.tile((2.0 ** -(np.arange(16) + 1.0))[None, :], (128, 1)).astype(np.float32)
    invf = (10000.0 ** (-(2.0 * (np.arange(128) % 32)) / 64.0)).astype(np.float32)[:, None]
    return dict(c_idf=idf, c_idb=idf.astype(bf), c_ones=np.ones((128, 128), bf), c_maskT=maskT.astype(bf),
                c_negm=negm, c_rot=rot.astype(bf), c_id240=(240.0 * idf).astype(bf), c_sel8=sel8, c_selh=selh, c_cvec=cvec, c_invf=invf)


def colmajor(v, nchunk):
    v = np.asarray(v, np.float32)
    return np.ascontiguousarray(np.swapaxes(v.reshape(v.shape[:-1] + (nchunk, 128)), -1, -2))


def prep_shared(inp, depth):
    f = np.float32
    fm, small, tm = perm_w_in()
    w_in = np.asarray(inp['w_in'], f)[:depth]
    wA = np.zeros((depth, D, NA), f)
    wA[:, :, 0:6656] = w_in[:, :, fm]
    wA[:, :, 6656:6656 + len(small)] = w_in[:, :, small]
    wA[:, :, 6784:] = w_in[:, :, tm]

    def bd(w):
        o = np.zeros((depth, 4, 128, 128), f)
        w = np.asarray(w, f)[:depth]
        for n in range(8):
            o[:, n // 2, (n % 2) * 64:(n % 2 + 1) * 64, (n % 2) * 64:(n % 2 + 1) * 64] = w[:, n]
        return o

    def chcol(v, nch):
        return np.ascontiguousarray(np.asarray(v, f)[:depth].reshape(depth, nch, 128, 1))

    def chtap(w, nch):
        w = np.asarray(w, f)[:depth]
        K = w.shape[1]
        return np.ascontiguousarray(w.reshape(depth, K, nch, 128).transpose(0, 2, 3, 1))

    d = dict(
        wA=wA, gmix=colmajor(inp['norm_mix'][:depth], 8), gffn=colmajor(inp['norm_ffn'][:depth], 8),
        gfin=colmajor(inp['norm_final'], 8),
        mcw=chtap(inp['mlstm_conv_w'], 8), mcb=chcol(inp['mlstm_conv_b'], 8),
        mib=np.asarray(inp['mlstm_i_bias'], f)[:depth].reshape(depth, 4, 1),
        mfb=np.asarray(inp['mlstm_f_bias'], f)[:depth].reshape(depth, 4, 1),
        mng=np.asarray(inp['mlstm_norm'], f)[:depth].reshape(depth, 4, 128),
        rcw=chtap(inp['rglru_conv_w'], 4), rcb=chcol(inp['rglru_conv_b'], 4),
        rwa=bd(inp['rglru_w_a']), rwx=bd(inp['rglru_w_x']),
        rba=chcol(inp['rglru_b_a'], 4), rbx=chcol(inp['rglru_b_x'], 4), rlam=chcol(inp['rglru_lambda'], 4),
        wbr=np.ascontiguousarray(np.asarray(inp['w_branch'], f)[:depth].reshape(depth, 1536, D)),
        wo=np.asarray(inp['w_out'], f)[:depth], fup=np.asarray(inp['ffn_up'], f)[:depth],
        fcw=np.ascontiguousarray(chtap(inp['ffn_conv_w'], 24).transpose(0, 2, 1, 3)), fcb=colmajor(np.asarray(inp['ffn_conv_b'])[:depth], 24),
        fdn=np.asarray(inp['ffn_down'], f)[:depth],
    )
    d.update(make_consts())
    return d


def kernel(**inputs):
    x = np.asarray(inputs['x'], np.float32)
    pos = np.asarray(inputs['positions'], np.int32)
    B, S, _ = x.shape
    depth = inputs['w_in'].shape[0]
    nc = build(S, depth)
    shared = prep_shared(inputs, depth)
    in_maps = []
    for c in range(8):
        b = c % B
        m = dict(shared)
        m['x'] = np.ascontiguousarray(x[b])
        m['pos'] = np.ascontiguousarray(pos[b:b + 1])
        in_maps.append(m)
    res = run_bass_kernel_spmd(nc, in_maps, core_ids=list(range(8)))
    return np.stack([np.asarray(res.results[b]['out'], np.float32) for b in range(B)], axis=0)
```

```python
import contextlib
import math
import numpy as np
import ml_dtypes
import concourse.bass as bass
import concourse.mybir as mybir
from concourse.bass_utils import run_bass_kernel_spmd
from concourse.alu_op_type import AluOpType as ALU

F32 = mybir.dt.float32
BF16 = mybir.dt.bfloat16
I32 = mybir.dt.int32
AF = mybir.ActivationFunctionType
AX = mybir.AxisListType

D = 1024
DB = 512
DFF = 3072
EPS = 1e-6
ENGS = ['pe', 'act', 'dve', 'pool', 'sp']
NBIS = 12
import os
SAFE_SAME_ENGINE = os.environ.get('SAFE_SAME', '1') == '1'


class Buf:
    __slots__ = ('t', 'w', 'r', 'dsem', 'dcnt', 'name')

    def __init__(self, t, name):
        self.t = t
        self.w = None
        self.r = {}
        self.dsem = None
        self.dcnt = 0
        self.name = name

    def __getitem__(self, k):
        return self.t[k]


class Prog:
    def __init__(self, nc):
        self.nc = nc
        self.engs = {'pe': nc.tensor, 'act': nc.scalar, 'dve': nc.vector, 'pool': nc.gpsimd, 'sp': nc.sync}
        self.sem = {e: nc.alloc_semaphore(name='s_' + e) for e in ENGS}
        self.cnt = {e: 0 for e in ENGS}
        self.known = {e: {} for e in ENGS}
        self.bufs = []
        self.dbufs = []
        self.pool = []
        for i in range(120):
            try:
                self.pool.append([nc.alloc_semaphore(name=f'dq{i}'), 0])
            except Exception:
                break
        self.free = list(range(len(self.pool)))
        self.ninst = 0
        self.nwait = 0

    def sb(self, es, name, shape, dt):
        self.uid = getattr(self, 'uid', 0) + 1
        name = f"{name}_{self.uid}"
        t = es.enter_context(self.nc.sbuf_tensor(name, list(shape), dt))
        b = Buf(t, name)
        self.bufs.append(b)
        return b

    def wrap(self, t, name):
        b = Buf(t, name)
        self.bufs.append(b)
        return b

    def _waits(self, e, deps):
        eng = self.engs[e]
        kn = self.known[e]
        for key, sem, val in deps:
            if kn.get(key, 0) >= val:
                continue
            kn[key] = val
            eng.wait_ge(sem, val)
            self.nwait += 1

    def _deps(self, e, reads, writes):
        deps = []
        for b in reads:
            if b.w is not None:
                deps.append(b.w)
        for b in writes:
            if b.w is not None:
                deps.append(b.w)
            deps.extend(b.r.values())
        if e == 'pe' or not SAFE_SAME_ENGINE:
            deps = [d for d in deps if d[0] != e]
        return deps

    def op(self, e, fn, reads=(), writes=()):
        self._waits(e, self._deps(e, reads, writes))
        self.cnt[e] += 1
        tok = (e, self.sem[e], self.cnt[e])
        fn(self.engs[e]).then_inc(self.sem[e], 1)
        self.ninst += 1
        for b in reads:
            b.r[e] = tok
        for b in writes:
            b.w = tok
            b.r = {}

    def dma(self, q, out_ap, in_ap, sbuf, reads=(), writes=()):
        self._waits(q, self._deps(q, reads, writes))
        if sbuf.dsem is None:
            sbuf.dsem = self.free.pop()
            self.dbufs.append(sbuf)
        pe_ = self.pool[sbuf.dsem]
        pe_[1] += 16
        key = ('d', sbuf.dsem)
        tok = (key, pe_[0], pe_[1])
        self.engs[q].dma_start(out=out_ap, in_=in_ap).then_inc(pe_[0], 16)
        self.ninst += 1
        for b in reads:
            b.r[key] = tok
        for b in writes:
            b.w = tok
            b.r = {}

    def load(self, buf, out_ap, in_ap, q='sp'):
        self.dma(q, out_ap, in_ap, buf, writes=(buf,))

    def store(self, buf, out_ap, in_ap, q='sp'):
        self.dma(q, out_ap, in_ap, buf, reads=(buf,))

    def barrier(self):
        toks = [(e, self.sem[e], self.cnt[e]) for e in ENGS if self.cnt[e] > 0]
        toks += [(('d', i), pe_[0], pe_[1]) for i, pe_ in enumerate(self.pool) if pe_[1] > 0]
        for e in ENGS:
            self._waits(e, [t for t in toks if t[0] != e])
        for b in self.bufs:
            b.w = None
            b.r = {}
        for b in self.dbufs:
            self.free.append(b.dsem)
            b.dsem = None
        self.dbufs = []


def MM(P, out_b, out_ap, lhs_b, lhsT, rhs_b, rhs, start, stop):
    P.op('pe', lambda e: e.matmul(out_ap, lhsT=lhsT, rhs=rhs, start=start, stop=stop),
         reads=(lhs_b, rhs_b), writes=(out_b,))


def TR(P, out_b, out_ap, in_b, in_ap, id_b, ident):
    P.op('pe', lambda e: e.transpose(out_ap, in_ap, ident), reads=(in_b, id_b), writes=(out_b,))


def ACT(P, out_b, out_ap, in_b, in_ap, func, bias=None, scale=None, accum=None, extra_r=(), extra_w=(), eng='act'):
    kw = {}
    if bias is not None:
        kw['bias'] = bias
    if scale is not None:
        kw['scale'] = scale
    if accum is not None:
        kw['accum_out'] = accum
    P.op('act', lambda e: e.activation(out=out_ap, in_=in_ap, func=func, **kw),
         reads=(in_b,) + tuple(extra_r), writes=(out_b,) + tuple(extra_w))


def TS(P, eng, out_b, out_ap, in_b, in_ap, s1, s2, op0, op1=None, accum=None, extra_r=(), extra_w=()):
    kw = {}
    if op1 is not None:
        kw['op1'] = op1
    if accum is not None:
        kw['accum_out'] = accum
    P.op(eng, lambda e: e.tensor_scalar(out=out_ap, in0=in_ap, scalar1=s1, scalar2=s2, op0=op0, **kw),
         reads=(in_b,) + tuple(extra_r), writes=(out_b,) + tuple(extra_w))


def TT(P, eng, out_b, out_ap, a_b, a_ap, b_b, b_ap, op):
    P.op(eng, lambda e: e.tensor_tensor(out=out_ap, in0=a_ap, in1=b_ap, op=op), reads=(a_b, b_b), writes=(out_b,))


def STT(P, out_b, out_ap, a_b, a_ap, scalar, b_b, b_ap, op0, op1, extra_r=()):
    P.op('dve', lambda e: e.scalar_tensor_tensor(out=out_ap, in0=a_ap, scalar=scalar, in1=b_ap, op0=op0, op1=op1),
         reads=(a_b, b_b) + tuple(extra_r), writes=(out_b,))


def CP(P, eng, out_b, out_ap, in_b, in_ap):
    if eng == 'act':
        P.op('act', lambda e: e.activation(out=out_ap, in_=in_ap, func=AF.Copy), reads=(in_b,), writes=(out_b,))
    else:
        P.op(eng, lambda e: e.tensor_copy(out=out_ap, in_=in_ap), reads=(in_b,), writes=(out_b,))


def MEMSET(P, eng, b, ap, val):
    P.op(eng, lambda e: e.memset(ap, val), writes=(b,))


SPL = np.cumsum([0, 1024, 512, 512, 4, 4, 512, 512, 512, 512, 512, 512, 64, 8, 3072])
NA = 6656 + 128 + 1536


def perm_w_in():
    s = SPL
    r = lambda a, b: list(range(a, b))
    fm = r(s[0], s[1]) + r(s[5], s[6]) + r(s[6], s[7]) + r(s[7], s[8]) + r(s[8], s[9]) + r(s[10], s[11]) + r(s[13], s[14])
    small = r(s[11], s[12]) + r(s[3], s[4]) + r(s[4], s[5]) + r(s[12], s[13])
    tm = r(s[1], s[2]) + r(s[2], s[3]) + r(s[9], s[10])
    return fm, small, tm


def build(S, depth, dbg=(), stop=99):
    nc = bass.Bass("TRN2", target_bir_lowering=False)
    NC = S // 128
    NT = S // 512
    P = Prog(nc)
    outs = {}

    def din(name, shape, dt=F32):
        return nc.dram_tensor(name, list(shape), dt, kind="ExternalInput").ap()

    def dscr(name, shape, dt):
        kind = "ExternalOutput" if name in dbg else "Internal"
        return nc.dram_tensor(name, list(shape), dt, kind=kind).ap()

    x_in = din("x", [S, D])
    pos_in = din("pos", [1, S], I32)
    wA_in = din("wA", [depth, D, NA])
    gmix_in = din("gmix", [depth, 128, 8])
    gffn_in = din("gffn", [depth, 128, 8])
    gfin_in = din("gfin", [128, 8])
    mcw_in = din("mcw", [depth, 8, 128, 4])
    mcb_in = din("mcb", [depth, 8, 128, 1])
    mib_in = din("mib", [depth, 4, 1])
    mfb_in = din("mfb", [depth, 4, 1])
    mng_in = din("mng", [depth, 4, 128])
    rcw_in = din("rcw", [depth, 4, 128, 4])
    rcb_in = din("rcb", [depth, 4, 128, 1])
    rwa_in = din("rwa", [depth, 4, 128, 128])
    rwx_in = din("rwx", [depth, 4, 128, 128])
    rba_in = din("rba", [depth, 4, 128, 1])
    rbx_in = din("rbx", [depth, 4, 128, 1])
    rlam_in = din("rlam", [depth, 4, 128, 1])
    wbr_in = din("wbr", [depth, 1536, D])
    wo_in = din("wo", [depth, D, D])
    fup_in = din("fup", [depth, D, 2 * DFF])
    fcw_in = din("fcw", [depth, 128, 24, 3])
    fcb_in = din("fcb", [depth, 128, 24])
    fdn_in = din("fdn", [depth, DFF, D])
    c_idf = din("c_idf", [128, 128])
    c_idb = din("c_idb", [128, 128], BF16)
    c_ones = din("c_ones", [128, 128], BF16)
    c_maskT = din("c_maskT", [128, 128], BF16)
    c_negm = din("c_negm", [128, 128])
    c_rot = din("c_rot", [128, 128], BF16)
    c_id240 = din("c_id240", [128, 128], BF16)
    c_sel8 = din("c_sel8", [8, 4, 128])
    c_selh = din("c_selh", [4, 4, 128])
    c_cvec = din("c_cvec", [128, 16])
    c_invf = din("c_invf", [128, 1])
    out_d = nc.dram_tensor("out", [S, D], F32, kind="ExternalOutput").ap()

    xT = dscr("xT", [D, S], F32)
    wA_b = dscr("wA_b", [D, NA], BF16)
    wbr_b = dscr("wbr_b", [1536, D], BF16)
    wo_b = dscr("wo_b", [D, D], BF16)
    fup_b = dscr("fup_b", [D, 2 * DFF], BF16)
    fdn_b = dscr("fdn_b", [DFF, D], BF16)
    fmT = dscr("fmT", [6656, S], BF16)
    smT = dscr("smT", [128, S], F32)
    tmv = dscr("tmv", [S, 1536], BF16)
    yT = dscr("yT", [1536, S], BF16)
    cosT = dscr("cosT", [128, S], F32)
    sinT = dscr("sinT", [128, S], F32)
    qrT = dscr("qrT", [512, S], BF16)
    qiT = dscr("qiT", [512, S], BF16)
    kiT = dscr("kiT", [64, S], BF16)
    kblk = dscr("kblk", [NC, 128, 4, 128], BF16)
    vblk = dscr("vblk", [NC, 128, 8, 128], BF16)
    sgn_d = dscr("sgn_d", [S, 8], F32)
    FM_QK, FM_RX, FM_RG, FM_AQ, FM_AK, FM_QI, FM_G = 0, 1024, 1536, 2048, 2560, 3072, 3584

    with contextlib.ExitStack() as ges:
        idf = P.sb(ges, "idf", [128, 128], F32)
        idb = P.sb(ges, "idb", [128, 128], BF16)
        onesb = P.sb(ges, "onesb", [128, 128], BF16)
        maskT = P.sb(ges, "maskT", [128, 128], BF16)
        negm = P.sb(ges, "negm", [128, 128], F32)
        rot = P.sb(ges, "rot", [128, 128], BF16)
        id240 = P.sb(ges, "id240", [128, 128], BF16)
        sel8 = P.sb(ges, "sel8", [8, 4, 128], F32)
        selh = P.sb(ges, "selh", [4, 4, 128], F32)
        cvec = P.sb(ges, "cvec", [128, 16], F32)
        invf = P.sb(ges, "invf", [128, 1], F32)
        epsc = P.sb(ges, "epsc", [128, 1], F32)
        onec = P.sb(ges, "onec", [128, 1], F32)
        for b, src in ((idf, c_idf), (idb, c_idb), (onesb, c_ones), (maskT, c_maskT), (negm, c_negm), (rot, c_rot), (id240, c_id240),
                       (sel8, c_sel8), (selh, c_selh), (cvec, c_cvec), (invf, c_invf)):
            P.load(b, b[:], src)
        MEMSET(P, 'dve', epsc, epsc[:], EPS)
        MEMSET(P, 'dve', onec, onec[:], 1.0)
        psum_t = ges.enter_context(nc.psum_tensor("ps", [128, 8, 512], F32))
        PS = [P.wrap(psum_t, f"ps{i}") for i in range(8)]

        def bank(i):
            return PS[i], psum_t[:, i, :]

        def bank2(i):
            return PS[i], psum_t[:, i:i + 2, :]

        def stage_transpose_in():
            with contextlib.ExitStack() as es:
                xin = [P.sb(es, f"xin{i}", [128, D], F32) for i in range(2)]
                xo = [P.sb(es, f"xo{i}", [128, 8, 128], F32) for i in range(2)]
                for i in range(NC):
                    a = xin[i % 2]
                    o = xo[i % 2]
                    P.load(a, a[:], x_in[i * 128:(i + 1) * 128, :])
                    for hlf in range(2):
                        pb, pa = bank(2 * (i % 2) + hlf)
                        for c in range(4):
                            cc = hlf * 4 + c
                            TR(P, pb, pa[:, c * 128:(c + 1) * 128], a, a[:, cc * 128:(cc + 1) * 128], idf, idf[:])
                        CP(P, 'dve' if hlf == 0 else 'act', o, o[:, hlf * 4:(hlf + 1) * 4, :], pb,
                           pa.rearrange("p (c t) -> p c t", c=4))
                    P.store(o, xT[:, i * 128:(i + 1) * 128].rearrange("(c p) t -> p c t", p=128), o[:])
                P.barrier()

        def convert(es_name, src, dst, K, N, gsrc=None):
            with contextlib.ExitStack() as es:
                W = 2048
                a = [P.sb(es, f"cv_a{i}", [128, W], F32) for i in range(3)]
                o = [P.sb(es, f"cv_o{i}", [128, W], BF16) for i in range(3)]
                g = None
                if gsrc is not None:
                    g = P.sb(es, "cv_g", [128, K // 128], F32)
                    P.load(g, g[:], gsrc)
                k = 0
                for kc in range(K // 128):
                    for n0 in range(0, N, W):
                        w = min(W, N - n0)
                        ai, oi = a[k % 3], o[k % 3]
                        P.load(ai, ai[:, :w], src[kc * 128:(kc + 1) * 128, n0:n0 + w])
                        eng = 'act' if k % 2 == 0 else 'dve'
                        if g is None:
                            CP(P, eng, oi, oi[:, :w], ai, ai[:, :w])
                        elif eng == 'act':
                            ACT(P, oi, oi[:, :w], ai, ai[:, :w], AF.Copy, scale=g[:, kc:kc + 1], extra_r=(g,))
                        else:
                            TS(P, 'dve', oi, oi[:, :w], ai, ai[:, :w], g[:, kc:kc + 1], None, ALU.mult, extra_r=(g,))
                        P.store(oi, dst[kc * 128:(kc + 1) * 128, n0:n0 + w], oi[:, :w])
                        k += 1
                P.barrier()

        def rmsnorm_tile(es_bufs, t0, T, gcol=None):
            xt, sq, hT, rstd = es_bufs
            P.load(xt, xt[:, :, :T], xT[:, t0:t0 + T].rearrange("(c p) t -> p c t", p=128))
            ACT(P, sq, sq[:, :, :T], xt, xt[:, :, :T], AF.Square)
            for hf in range(T // 512):
                pb, pa = bank(6 + hf % 2)
                for c in range(8):
                    MM(P, pb, pa, onesb, onesb[:], sq, sq[:, c, hf * 512:(hf + 1) * 512], c == 0, c == 7)
                ACT(P, rstd, rstd[:, hf * 512:(hf + 1) * 512], pb, pa, AF.Sqrt, bias=epsc[:], scale=1.0 / D, extra_r=(epsc,))
            P.op('dve', lambda e: e.reciprocal(rstd[:, :T], rstd[:, :T]), reads=(rstd,), writes=(rstd,))
            if gcol is None:
                TT(P, 'dve', hT, hT[:, :, :T], xt, xt[:, :, :T], rstd, rstd[:, :T].unsqueeze(1).to_broadcast([128, 8, T]), ALU.mult)
            else:
                for c in range(8):
                    STT(P, hT, hT[:, c, :T], xt, xt[:, c, :T], gcol[:, c:c + 1], rstd, rstd[:, :T], ALU.mult, ALU.mult, extra_r=(gcol,))
            return xt, hT

        def stage_inproj():
            T = 1024 if S >= 1024 else S
            with contextlib.ExitStack() as es:
                nb = (P.sb(es, "A_xt", [128, 8, T], F32), P.sb(es, "A_sq", [128, 8, T], BF16),
                      P.sb(es, "A_hT", [128, 8, T], BF16), P.sb(es, "A_rstd", [128, T], F32))
                wb = [P.sb(es, f"A_w{i}", [128, 8, 512], BF16) for i in range(3)]
                ob = [P.sb(es, f"A_o{i}", [128, T], BF16) for i in range(4)]
                osm = P.sb(es, "A_osm", [128, T], F32)
                otm = [P.sb(es, f"A_ot{i}", [128, 512], BF16) for i in range(3)]
                nh = T // 512
                wk = 0
                ok = 0
                pk = 0
                for t0 in range(0, S, T):
                    xt, hT = rmsnorm_tile(nb, t0, T)
                    for blk in range(13):
                        w = wb[wk % 3]
                        wk += 1
                        P.load(w, w[:], wA_b[:, blk * 512:(blk + 1) * 512].rearrange("(c p) n -> p c n", p=128))
                        for ch in range(4):
                            slot = (pk % 3) * 2
                            pk += 1
                            pb = PS[slot]
                            for hf in range(nh):
                                for c in range(8):
                                    MM(P, pb, psum_t[:, slot + hf, :], w, w[:, c, ch * 128:(ch + 1) * 128], hT,
                                       hT[:, c, hf * 512:(hf + 1) * 512], c == 0, c == 7)
                            o = ob[ok % 4]
                            CP(P, 'act' if ok % 2 == 0 else 'dve', o, o[:, :].rearrange("p (h t) -> p h t", h=nh), pb,
                               psum_t[:, slot:slot + nh, :])
                            ok += 1
                            r0 = blk * 512 + ch * 128
                            P.store(o, fmT[r0:r0 + 128, t0:t0 + T], o[:])
                    w = wb[wk % 3]
                    wk += 1
                    P.load(w, w[:, :, 0:128], wA_b[:, 6656:6784].rearrange("(c p) n -> p c n", p=128))
                    slot = (pk % 3) * 2
                    pk += 1
                    pb = PS[slot]
                    for hf in range(nh):
                        for c in range(8):
                            MM(P, pb, psum_t[:, slot + hf, :], w, w[:, c, 0:128], hT, hT[:, c, hf * 512:(hf + 1) * 512], c == 0, c == 7)
                    CP(P, 'dve', osm, osm[:, :].rearrange("p (h t) -> p h t", h=nh), pb, psum_t[:, slot:slot + nh, :])
                    P.store(osm, smT[:, t0:t0 + T], osm[:])
                    for blk in range(3):
                        w = wb[wk % 3]
                        wk += 1
                        c0 = 6784 + blk * 512
                        P.load(w, w[:], wA_b[:, c0:c0 + 512].rearrange("(c p) n -> p c n", p=128))
                        for st in range(T // 128):
                            slot = (pk % 3) * 2
                            pk += 1
                            pb, pa = bank(slot)
                            for c in range(8):
                                MM(P, pb, pa, hT, hT[:, c, st * 128:(st + 1) * 128], w, w[:, c, :], c == 0, c == 7)
                            o = otm[ok % 3]
                            CP(P, 'act' if ok % 2 == 0 else 'dve', o, o[:], pb, pa)
                            ok += 1
                            P.store(o, tmv[t0 + st * 128:t0 + (st + 1) * 128, blk * 512:(blk + 1) * 512], o[:])
                P.barrier()

        def conv_fm(xp, acc, wcol, K, T, extra):
            TS(P, 'dve', acc, acc[:, :T], xp, xp[:, 0:T], wcol[:, 0:1], None, ALU.mult, extra_r=(wcol,))
            for j in range(1, K):
                STT(P, acc, acc[:, :T], xp, xp[:, j:j + T], wcol[:, j:j + 1], acc, acc[:, :T], ALU.mult, ALU.add, extra_r=(wcol,))

        def stage_mlstm(l):
            SEG = min(S, 2048)
            NCS = SEG // 128
            with contextlib.ExitStack() as es:
                tokQ = P.sb(es, "m_tokQ", [128, NC, 16], F32)
                decB = P.sb(es, "m_decB", [128, 4, NC], F32)
                with contextlib.ExitStack() as es2:
                    gi = P.sb(es2, "m_gi", [4, SEG], F32)
                    gf = P.sb(es2, "m_gf", [4, SEG], F32)
                    Gn = P.sb(es2, "m_Gn", [4, SEG], F32)
                    mt = P.sb(es2, "m_mt", [4, SEG], F32)
                    one4 = P.sb(es2, "m_one4", [4, SEG], F32)
                    tA = P.sb(es2, "m_tA", [4, SEG], F32)
                    Q1 = P.sb(es2, "m_Q1", [128, SEG], F32)
                    Q2 = P.sb(es2, "m_Q2", [128, SEG], F32)
                    mpe = P.sb(es2, "m_mpe", [4, NC + 1], F32)
                    dec = P.sb(es2, "m_dec", [4, NC], F32)
                    bi = P.sb(es2, "m_bi", [4, 1], F32)
                    bfn = P.sb(es2, "m_bfn", [4, 1], F32)
                    car = P.sb(es2, "m_car", [4, 2], F32)
                    P.load(bi, bi[:], mib_in[l])
                    P.load(bfn, bfn[:], mfb_in[l])
                    TS(P, 'dve', bfn, bfn[:], bfn, bfn[:], -1.0, None, ALU.mult)
                    MEMSET(P, 'dve', one4, one4[:], 1.0)
                    MEMSET(P, 'dve', Q1, Q1[:], 0.0)
                    MEMSET(P, 'dve', Q2, Q2[:], 0.0)
                    MEMSET(P, 'dve', mpe, mpe[:], 0.0)
                    MEMSET(P, 'dve', car, car[:], 0.0)
                    for sg in range(S // SEG):
                        t0 = sg * SEG
                        c0 = sg * NCS
                        P.load(gi, gi[:], smT[64:68, t0:t0 + SEG])
                        P.load(gf, gf[:], smT[68:72, t0:t0 + SEG])
                        ACT(P, gf, gf[:], gf, gf[:], AF.Exp, bias=bfn[:], scale=-1.0, extra_r=(bfn,))
                        ACT(P, gf, gf[:], gf, gf[:], AF.Ln, bias=onec[0:4, :], scale=1.0, extra_r=(onec,))
                        P.op('dve', lambda e: e.tensor_tensor_scan(out=Gn[:], data0=one4[:], data1=gf[:], initial=car[:, 0:1],
                                                                  op0=ALU.mult, op1=ALU.add), reads=(one4, gf, car), writes=(Gn,))
                        STT(P, gi, gi[:], gi, gi[:], bi[:, 0:1], Gn, Gn[:], ALU.add, ALU.add, extra_r=(bi,))
                        P.op('dve', lambda e: e.tensor_tensor_scan(out=mt[:], data0=one4[:], data1=gi[:], initial=car[:, 1:2],
                                                                  op0=ALU.mult, op1=ALU.max), reads=(one4, gi, car), writes=(mt,))
                        CP(P, 'dve', car, car[:, 0:1], Gn, Gn[:, SEG - 1:SEG])
                        CP(P, 'dve', car, car[:, 1:2], mt, mt[:, SEG - 1:SEG])
                        mt3 = mt[:].rearrange("p (c t) -> p c t", t=128)
                        CP(P, 'dve', mpe, mpe[:, c0 + 1:c0 + 1 + NCS], mt, mt3[:, :, 127])
                        mend_b = mpe[:, c0 + 1:c0 + 1 + NCS].unsqueeze(2).to_broadcast([4, NCS, 128])
                        mprev_b = mpe[:, c0:c0 + NCS].unsqueeze(2).to_broadcast([4, NCS, 128])
                        q3 = lambda b, p0: b[p0:p0 + 4, :].rearrange("p (c t) -> p c t", t=128)
                        tA3 = tA[:].rearrange("p (c t) -> p c t", t=128)
                        TT(P, 'dve', tA, tA3, mt, mt3, mpe, mend_b, ALU.subtract)
                        ACT(P, Q1, Q1[0:4, :], tA, tA[:], AF.Exp, scale=-1.0)
                        TT(P, 'dve', tA, tA3, mt, mt3, mpe, mprev_b, ALU.subtract)
                        ACT(P, Q1, Q1[32:36, :], tA, tA[:], AF.Exp, scale=-1.0)
                        TT(P, 'dve', tA, tA3, gi, gi[:].rearrange("p (c t) -> p c t", t=128), mpe, mend_b, ALU.subtract)
                        ACT(P, Q1, Q1[64:68, :], tA, tA[:], AF.Exp)
                        TT(P, 'dve', tA, tA[:], Gn, Gn[:], mt, mt[:], ALU.subtract)
                        ACT(P, Q2, Q2[0:4, :], tA, tA[:], AF.Exp)
                        for cg in range(NCS // 4):
                            pb, pa = bank(cg % 2)
                            pb2, pa2 = bank(2 + cg % 2)
                            for k in range(4):
                                c = cg * 4 + k
                                TR(P, pb, pa[:, k * 128:(k + 1) * 128], Q1, Q1[:, c * 128:(c + 1) * 128], idf, idf[:])
                                TR(P, pb2, pa2[:, k * 128:(k + 1) * 128], Q2, Q2[:, c * 128:(c + 1) * 128], idf, idf[:])
                            cc = c0 + cg * 4
                            CP(P, 'dve', tokQ, tokQ[:, cc:cc + 4, 0:12].rearrange("p c (j x) -> p c j x", j=3), pb,
                               pa.rearrange("p (c j x) -> p c j x", c=4, j=4)[:, :, 0:3, 0:4])
                            CP(P, 'act', tokQ, tokQ[:, cc:cc + 4, 12:16], pb2, pa2.rearrange("p (c x) -> p c x", c=4)[:, :, 0:4])
                    TT(P, 'dve', dec, dec[:], mpe, mpe[:, 0:NC], mpe, mpe[:, 1:NC + 1], ALU.subtract)
                    ACT(P, dec, dec[:], dec, dec[:], AF.Exp)
                    for h in range(4):
                        pb, pa = bank(4)
                        MM(P, pb, pa[:, 0:NC], selh, selh[:, h, :], dec, dec[:], True, True)
                        CP(P, 'dve', decB, decB[:, h, :], pb, pa[:, 0:NC])
                    P.barrier()
                TS(P, 'dve', tokQ, tokQ[:, :, 8:12], tokQ, tokQ[:, :, 8:12], 128.0 ** -0.5, None, ALU.mult)
                qT = P.sb(es, "m_qT", [128, S], BF16)
                kT = P.sb(es, "m_kT", [128, S], BF16)
                vx = P.sb(es, "m_vx", [128, NC, 129], BF16)
                Hall = P.sb(es, "m_H", [128, NC, 129], F32)
                scr = P.sb(es, "m_scr", [128, S], F32)
                yb = P.sb(es, "m_yb", [128, S + 8], BF16)
                wc = P.sb(es, "m_wc", [128, 4], F32)
                bc = P.sb(es, "m_bc", [128, 1], F32)
                gB = P.sb(es, "m_gB", [128, 128], F32)
                CTf = P.sb(es, "m_CTf", [128, 129], F32)
                CTb = P.sb(es, "m_CTb", [128, 129], BF16)
                Sm = [P.sb(es, f"m_Sm{i}", [128, 128], BF16) for i in range(2)]
                kp = [P.sb(es, f"m_kp{i}", [128, 128], BF16) for i in range(2)]
                st = P.sb(es, "m_st", [128, NC, 4], F32)
                scr3 = scr[:].rearrange("p (c d) -> p c d", d=128)
                for h in range(4):
                    for which, dstb in ((h, qT), (4 + h, kT)):
                        P.load(wc, wc[:], mcw_in[l, which])
                        P.load(bc, bc[:], mcb_in[l, which])
                        MEMSET(P, 'pool', yb, yb[:, 0:3], 0.0)
                        P.load(yb, yb[:, 3:3 + S], fmT[FM_QK + which * 128:FM_QK + (which + 1) * 128, :])
                        conv_fm(yb, scr, wc, 4, S, ())
                        ACT(P, dstb, dstb[:], scr, scr[:], AF.Silu, bias=bc[:], extra_r=(bc,))
                    MEMSET(P, 'pool', vx, vx[:, :, 128:129], 1.0)
                    P.load(vx, vx[:, :, 0:128], tmv[:, h * 128:(h + 1) * 128].rearrange("(c p) d -> p c d", p=128))
                    P.load(gB, gB[:], mng_in[l, h:h + 1, :].partition_broadcast(128))
                    MEMSET(P, 'dve', CTf, CTf[:], 0.0)
                    MEMSET(P, 'pool', CTb, CTb[:], 0.0)
                    for c in range(NC):
                        cs = slice(c * 128, (c + 1) * 128)
                        sm, kpp = Sm[c % 2], kp[c % 2]
                        b0, a0 = bank((c % 2) * 4 + 0)
                        b1, a1 = bank((c % 2) * 4 + 1)
                        b2, a2 = bank((c % 2) * 4 + 2)
                        b3, a3 = bank((c % 2) * 4 + 3)
                        MM(P, b0, a0[:, 0:128], kT, kT[:, cs], qT, qT[:, cs], True, True)
                        STT(P, sm, sm[:], b0, a0[:, 0:128], tokQ[:, c, 8 + h:9 + h], maskT, maskT[:], ALU.mult, ALU.mult, extra_r=(tokQ,))
                        MM(P, b1, a1[:, 0:129], sm, sm[:], vx, vx[:, c, :], True, True)
                        MM(P, b2, a2[:, 0:129], qT, qT[:, cs], CTb, CTb[:], True, True)
                        TS(P, 'dve', Hall, Hall[:, c, :], b1, a1[:, 0:129], tokQ[:, c, h:h + 1], None, ALU.mult, extra_r=(tokQ,))
                        STT(P, Hall, Hall[:, c, :], b2, a2[:, 0:129], tokQ[:, c, 4 + h:5 + h], Hall, Hall[:, c, :], ALU.mult, ALU.add, extra_r=(tokQ,))
                        kpb = psum_t[:, (c % 2) * 4 + 0, 256:384].bitcast(BF16)
                        TR(P, b0, kpb[:, 0:128], kT, kT[:, cs], idb, idb[:])
                        TS(P, 'dve', kpp, kpp[:], b0, kpb[:, 0:128], tokQ[:, c, 8 + h:9 + h], None, ALU.mult, extra_r=(tokQ,))
                        MM(P, b3, a3[:, 0:129], kpp, kpp[:], vx, vx[:, c, :], True, True)
                        STT(P, CTf, CTf[:], CTf, CTf[:], decB[:, h, c:c + 1], b3, a3[:, 0:129], ALU.mult, ALU.add, extra_r=(decB,))
                        CP(P, 'act', CTb, CTb[:], CTf, CTf[:])
                    den = Hall[:, :, 128]
                    STT(P, st, st[:, :, 0], Hall, den, -1.0, Hall, den, ALU.mult, ALU.max)
                    TT(P, 'dve', st, st[:, :, 0], st, st[:, :, 0], tokQ, tokQ[:, :, 12 + h], ALU.max)
                    P.op('dve', lambda e: e.reciprocal(st[:, :, 0], st[:, :, 0]), reads=(st,), writes=(st,))
                    H3 = Hall[:, :, 0:128]
                    TT(P, 'dve', Hall, H3, Hall, H3, st, st[:, :, 0:1].to_broadcast([128, NC, 128]), ALU.mult)
                    P.op('dve', lambda e: e.tensor_reduce(out=st[:, :, 1], in_=H3, axis=AX.X, op=ALU.add), reads=(Hall,), writes=(st,))
                    TS(P, 'dve', st, st[:, :, 1], st, st[:, :, 1], 1.0 / 128, None, ALU.mult)
                    TT(P, 'dve', Hall, H3, Hall, H3, st, st[:, :, 1:2].to_broadcast([128, NC, 128]), ALU.subtract)
                    TT(P, 'pool', scr, scr3, Hall, H3, Hall, H3, ALU.mult)
                    P.op('dve', lambda e: e.tensor_reduce(out=st[:, :, 2], in_=scr3, axis=AX.X, op=ALU.add), reads=(scr,), writes=(st,))
                    ACT(P, st, st[:, :, 2], st, st[:, :, 2], AF.Sqrt, bias=epsc[:], scale=1.0 / 128, extra_r=(epsc,))
                    P.op('dve', lambda e: e.reciprocal(st[:, :, 2], st[:, :, 2]), reads=(st,), writes=(st,))
                    TT(P, 'dve', Hall, H3, Hall, H3, st, st[:, :, 2:3].to_broadcast([128, NC, 128]), ALU.mult)
                    TT(P, 'dve', Hall, H3, Hall, H3, gB, gB[:].unsqueeze(1).to_broadcast([128, NC, 128]), ALU.mult)
                    kT3 = kT[:].rearrange("p (c d) -> p c d", d=128)
                    P.load(kT, kT3, tmv[:, 512 + h * 128:512 + (h + 1) * 128].rearrange("(c p) d -> p c d", p=128))
                    ACT(P, scr, scr3, kT, kT3, AF.Sigmoid)
                    yb3 = yb[:, 0:S].rearrange("p (c d) -> p c d", d=128)
                    TT(P, 'dve', yb, yb3, Hall, H3, scr, scr3, ALU.mult)
                    for cg in range(NC // 4):
                        pbk = PS[cg % 2]
                        pv = psum_t[:, cg % 2, 0:256].bitcast(BF16)
                        for k in range(4):
                            c = cg * 4 + k
                            TR(P, pbk, pv[:, k * 128:(k + 1) * 128], yb, yb[:, c * 128:(c + 1) * 128], idb, idb[:])
                        CP(P, 'act' if cg % 2 else 'dve', qT, qT[:, cg * 512:(cg + 1) * 512], pbk, pv[:, 0:512])
                    P.store(qT, yT[h * 128:(h + 1) * 128, :], qT[:])
                P.barrier()

        def stage_rglru(l):
            SEG = min(S, 2048)
            with contextlib.ExitStack() as es:
                wab = P.sb(es, "r_wab", [128, 128], BF16)
                wxb = P.sb(es, "r_wxb", [128, 128], BF16)
                wtmp = P.sb(es, "r_wtmp", [128, 128], F32)
                wc = P.sb(es, "r_wc", [128, 4], F32)
                cols = P.sb(es, "r_cols", [128, 8], F32)
                xp = P.sb(es, "r_xp", [128, SEG + 3], BF16)
                xc = P.sb(es, "r_xc", [128, SEG], F32)
                xcb = P.sb(es, "r_xcb", [128, SEG], BF16)
                rr = P.sb(es, "r_rr", [128, SEG], F32)
                ii = P.sb(es, "r_ii", [128, SEG], F32)
                aa = P.sb(es, "r_aa", [128, SEG], F32)
                hh = P.sb(es, "r_hh", [128, SEG], F32)
                rg = P.sb(es, "r_rg", [128, SEG], BF16)
                yo = P.sb(es, "r_yo", [128, SEG], BF16)
                car = P.sb(es, "r_car", [128, 1], F32)
                for ch in range(4):
                    P.load(wtmp, wtmp[:], rwa_in[l, ch])
                    CP(P, 'dve', wab, wab[:], wtmp, wtmp[:])
                    P.load(wtmp, wtmp[:], rwx_in[l, ch])
                    CP(P, 'dve', wxb, wxb[:], wtmp, wtmp[:])
                    P.load(wc, wc[:], rcw_in[l, ch])
                    P.load(cols, cols[:, 0:1], rcb_in[l, ch])
                    P.load(cols, cols[:, 1:2], rba_in[l, ch])
                    P.load(cols, cols[:, 2:3], rbx_in[l, ch])
                    P.load(cols, cols[:, 3:4], rlam_in[l, ch])
                    ACT(P, cols, cols[:, 3:4], cols, cols[:, 3:4], AF.Exp, scale=-1.0)
                    ACT(P, cols, cols[:, 3:4], cols, cols[:, 3:4], AF.Ln, bias=onec[:], scale=1.0, extra_r=(onec,))
                    TS(P, 'dve', cols, cols[:, 4:5], cols, cols[:, 3:4], -16.0, None, ALU.mult)
                    TS(P, 'dve', cols, cols[:, 3:4], cols, cols[:, 3:4], -8.0, None, ALU.mult)
                    MEMSET(P, 'dve', car, car[:], 0.0)
                    for sg in range(S // SEG):
                        t0 = sg * SEG
                        r0 = FM_RX + ch * 128
                        if sg == 0:
                            MEMSET(P, 'pool', xp, xp[:, 0:3], 0.0)
                            P.load(xp, xp[:, 3:3 + SEG], fmT[r0:r0 + 128, 0:SEG])
                        else:
                            P.load(xp, xp[:, 0:3 + SEG], fmT[r0:r0 + 128, t0 - 3:t0 + SEG])
                        P.load(rg, rg[:], fmT[FM_RG + ch * 128:FM_RG + (ch + 1) * 128, t0:t0 + SEG])
                        conv_fm(xp, xc, wc, 4, SEG, ())
                        TS(P, 'dve', xc, xc[:], xc, xc[:], cols[:, 0:1], None, ALU.add, extra_r=(cols,))
                        CP(P, 'act', xcb, xcb[:], xc, xc[:])
                        for q in range(SEG // 512):
                            qs = slice(q * 512, (q + 1) * 512)
                            pb, pa = bank(q % 4)
                            MM(P, pb, pa, wab, wab[:], xcb, xcb[:, qs], True, True)
                            ACT(P, rr, rr[:, qs], pb, pa, AF.Sigmoid, bias=cols[:, 1:2], extra_r=(cols,))
                            pb, pa = bank(4 + q % 4)
                            MM(P, pb, pa, wxb, wxb[:], xcb, xcb[:, qs], True, True)
                            ACT(P, ii, ii[:, qs], pb, pa, AF.Sigmoid, bias=cols[:, 2:3], extra_r=(cols,))
                        ACT(P, aa, aa[:], rr, rr[:], AF.Exp, scale=cols[:, 3:4], extra_r=(cols,))
                        ACT(P, rr, rr[:], rr, rr[:], AF.Exp, scale=cols[:, 4:5], extra_r=(cols,))
                        TS(P, 'dve', rr, rr[:], rr, rr[:], -1.0, 1.0, ALU.mult, ALU.add)
                        ACT(P, rr, rr[:], rr, rr[:], AF.Sqrt)
                        TT(P, 'dve', ii, ii[:], ii, ii[:], xc, xc[:], ALU.mult)
                        TT(P, 'dve', ii, ii[:], ii, ii[:], rr, rr[:], ALU.mult)
                        P.op('dve', lambda e: e.tensor_tensor_scan(out=hh[:], data0=aa[:], data1=ii[:], initial=car[:, 0:1],
                                                                  op0=ALU.mult, op1=ALU.add), reads=(aa, ii, car), writes=(hh,))
                        CP(P, 'dve', car, car[:], hh, hh[:, SEG - 1:SEG])
                        ACT(P, xc, xc[:], rg, rg[:], AF.Gelu_apprx_tanh)
                        TT(P, 'dve', yo, yo[:], xc, xc[:], hh, hh[:], ALU.mult)
                        P.store(yo, yT[512 + ch * 128:512 + (ch + 1) * 128, t0:t0 + SEG], yo[:])
                P.barrier()

        def stage_rope_tables():
            T = min(S, 2048)
            C1 = 6.28125
            C2 = float(np.float32(round((2 * math.pi - C1) * 2 ** 20) / 2 ** 20))
            C3 = float(2 * math.pi - C1 - C2)
            MAGIC = 12582912.0
            with contextlib.ExitStack() as es:
                pi_ = P.sb(es, "t_pi", [128, T], I32)
                ang = P.sb(es, "t_ang", [128, T], F32)
                kf = P.sb(es, "t_kf", [128, T], F32)
                sn = P.sb(es, "t_sn", [128, T], F32)
                cs = P.sb(es, "t_cs", [128, T], F32)
                for t0 in range(0, S, T):
                    P.load(pi_, pi_[:], pos_in[0:1, t0:t0 + T].partition_broadcast(128))
                    CP(P, 'dve', ang, ang[:], pi_, pi_[:])
                    TS(P, 'dve', ang, ang[:], ang, ang[:], invf[:, 0:1], None, ALU.mult, extra_r=(invf,))
                    TS(P, 'dve', kf, kf[:], ang, ang[:], 1.0 / (2 * math.pi), MAGIC, ALU.mult, ALU.add)
                    TS(P, 'dve', kf, kf[:], kf, kf[:], MAGIC, None, ALU.subtract)
                    for cc in (C1, C2, C3):
                        STT(P, ang, ang[:], kf, kf[:], -cc, ang, ang[:], ALU.mult, ALU.add)
                    TS(P, 'dve', ang, ang[:], ang, ang[:], 3.1415925, -3.1415925, ALU.min, ALU.max)
                    ACT(P, sn, sn[:], ang, ang[:], AF.Sin)
                    ACT(P, cs, cs[:], ang, ang[:], AF.Sin, scale=0.5)
                    TT(P, 'dve', cs, cs[:], cs, cs[:], cs, cs[:], ALU.mult)
                    TS(P, 'dve', cs, cs[:], cs, cs[:], -2.0, 1.0, ALU.mult, ALU.add)
                    P.store(sn, sinT[:, t0:t0 + T], sn[:])
                    P.store(cs, cosT[:, t0:t0 + T], cs[:])
                P.barrier()

        def stage_rope(l):
            T = 512
            with contextlib.ExitStack() as es:
                cs = [P.sb(es, f"p_cs{i}", [128, T], F32) for i in range(2)]
                sn = [P.sb(es, f"p_sn{i}", [128, T], F32) for i in range(2)]
                a = [P.sb(es, f"p_a{i}", [128, T], BF16) for i in range(3)]
                t1 = [P.sb(es, f"p_t1{i}", [128, T], F32) for i in range(2)]
                t2 = [P.sb(es, f"p_t2{i}", [128, T], F32) for i in range(2)]
                o = [P.sb(es, f"p_o{i}", [128, T], BF16) for i in range(3)]
                sm = [P.sb(es, f"p_sm{i}", [128, T], F32) for i in range(2)]
                w8 = [P.sb(es, f"p_w8{i}", [128, T], F32) for i in range(2)]
                w8a = [P.sb(es, f"p_w8a{i}", [8, T], F32) for i in range(2)]
                for i in range(2):
                    MEMSET(P, 'dve', w8[i], w8[i][:], 0.0)
                kib = P.sb(es, "p_kib", [128, T], BF16)
                sg = [P.sb(es, f"p_sg{i}", [128, 4, 8], F32) for i in range(2)]
                vt = [P.sb(es, f"p_vt{i}", [128, 512], BF16) for i in range(2)]
                vx = [P.sb(es, f"p_vx{i}", [128, 8, 128], BF16) for i in range(2)]
                for i in range(2):
                    MEMSET(P, 'pool', vx[i], vx[i][:], 1.0)
                k = 0
                for ti, t0 in enumerate(range(0, S, T)):
                    c_, s_ = cs[ti % 2], sn[ti % 2]
                    P.load(c_, c_[:], cosT[:, t0:t0 + T])
                    P.load(s_, s_[:], sinT[:, t0:t0 + T])
                    w = w8[ti % 2]
                    wa = w8a[ti % 2]
                    P.load(w, w[0:8, :], smT[72:80, t0:t0 + T])
                    STT(P, wa, wa[:], w, w[0:8, :], -1.0, w, w[0:8, :], ALU.mult, ALU.max)
                    sgb = sg[ti % 2]
                    pb, pa = bank(7)
                    for j in range(4):
                        TR(P, pb, pa[:, j * 128:(j + 1) * 128], w, w[:, j * 128:(j + 1) * 128], idf, idf[:])
                    ACT(P, sgb, sgb[:], pb, pa.rearrange("p (j x) -> p j x", j=4)[:, :, 0:8], AF.Sign)
                    P.store(sgb, sgn_d[t0:t0 + T, :].rearrange("(j p) h -> p j h", p=128), sgb[:])

                    def rope_chunk(src_b, src_ap, scale_bank=None):
                        nonlocal k
                        t1b, t2b, ob = t1[k % 2], t2[k % 2], o[k % 3]
                        pb, pa = bank(k % 4)
                        MM(P, pb, pa, rot, rot[:], src_b, src_ap, True, True)
                        TT(P, 'dve', t1b, t1b[:], src_b, src_ap, c_, c_[:], ALU.mult)
                        TT(P, 'dve', t2b, t2b[:], pb, pa, s_, s_[:], ALU.mult)
                        if scale_bank is None:
                            TT(P, 'dve', ob, ob[:], t1b, t1b[:], t2b, t2b[:], ALU.add)
                        else:
                            TT(P, 'pool', t1b, t1b[:], t1b, t1b[:], t2b, t2b[:], ALU.add)
                            TT(P, 'dve', ob, ob[:], scale_bank[0], scale_bank[1], t1b, t1b[:], ALU.mult)
                        k += 1
                        return ob

                    for ch in range(4):
                        ab = a[k % 3]
                        P.load(ab, ab[:], fmT[FM_AQ + ch * 128:FM_AQ + (ch + 1) * 128, t0:t0 + T])
                        ob = rope_chunk(ab, ab[:])
                        P.store(ob, qrT[ch * 128:(ch + 1) * 128, t0:t0 + T], ob[:])
                    for ch in range(4):
                        ab = a[k % 3]
                        P.load(ab, ab[:], fmT[FM_AK + ch * 128:FM_AK + (ch + 1) * 128, t0:t0 + T])
                        ob = rope_chunk(ab, ab[:])
                        P.store(ob, kblk[t0 // 128:t0 // 128 + 4, :, ch, :].rearrange("j p s -> p j s"),
                                ob[:].rearrange("p (j s) -> p j s", j=4))
                    for ch in range(4):
                        ab = a[k % 3]
                        P.load(ab, ab[:], fmT[FM_QI + ch * 128:FM_QI + (ch + 1) * 128, t0:t0 + T])
                        pbw, paw = bank(4 + ch % 2)
                        MM(P, pbw, paw, sel8, sel8[:, ch, :], wa, wa[:], True, True)
                        ob = rope_chunk(ab, ab[:], scale_bank=(pbw, paw))
                        P.store(ob, qiT[ch * 128:(ch + 1) * 128, t0:t0 + T], ob[:])
                    smb = sm[ti % 2]
                    P.load(smb, smb[0:64, :], smT[0:64, t0:t0 + T])
                    MEMSET(P, 'pool', kib, kib[:], 0.0)
                    CP(P, 'act', kib, kib[0:64, :], smb, smb[0:64, :])
                    ob = rope_chunk(kib, kib[:])
                    P.store(ob, kiT[:, t0:t0 + T], ob[0:64, :])
                    for j in range(4):
                        vtb, vxb = vt[j % 2], vx[j % 2]
                        tt = t0 + j * 128
                        P.load(vtb, vtb[:], tmv[tt:tt + 128, 1024:1536])
                        v4 = vtb[:].rearrange("p (h two d) -> p h two d", two=2, d=64)
                        x4 = vxb[:].rearrange("p (h two) d -> p h two d", two=2)
                        CP(P, 'dve', vxb, x4[:, :, 0, 0:64], vtb, v4[:, :, 0, :])
                        CP(P, 'dve', vxb, x4[:, :, 1, 64:128], vtb, v4[:, :, 1, :])
                        P.store(vxb, vblk[tt // 128], vxb[:])
                P.barrier()

        def stage_dsa(l):
            with contextlib.ExitStack() as es:
                ki2 = P.sb(es, "d_ki2", [128, S], BF16)
                Irow = P.sb(es, "d_Irow", [128, S], F32)
                junk = P.sb(es, "d_junk", [128, S], BF16)
                mrow = P.sb(es, "d_mrow", [128, S], BF16)
                junk2 = P.sb(es, "d_junk2", [128, S // 2 + 128], BF16)
                mT = [P.sb(es, f"d_mT{i}", [128, NC, 128], BF16) for i in range(2)]
                rl = [P.sb(es, f"d_rl{i}", [128, 2, 512], BF16) for i in range(3)]
                Eb = [P.sb(es, f"d_E{i}", [128, 8, 128], BF16) for i in range(3)]
                kb = [P.sb(es, f"d_kb{i}", [128, 4, 128], BF16) for i in range(3)]
                vb = [P.sb(es, f"d_vb{i}", [128, 8, 128], BF16) for i in range(3)]
                qi_t = [P.sb(es, f"d_qi{i}", [128, 4, 128], BF16) for i in range(2)]
                q_t = [P.sb(es, f"d_q{i}", [128, 4, 128], BF16) for i in range(2)]
                sg_t = [P.sb(es, f"d_sg{i}", [128, 8], F32) for i in range(2)]
                Dall = [P.sb(es, f"d_D{i}", [128, 8, 128], BF16) for i in range(2)]
                bs = [P.sb(es, f"d_bs{i}", [128, 24], F32) for i in range(2)]
                bsa = [P.sb(es, f"d_bsa{i}", [128, 2], F32) for i in range(2)]
                osb = [P.sb(es, f"d_os{i}", [128, 8, 128], F32) for i in range(2)]
                rcp = [P.sb(es, f"d_rc{i}", [128, 8, 128], F32) for i in range(2)]
                yc = [P.sb(es, f"d_yc{i}", [128, 4, 128], BF16) for i in range(2)]
                P.load(ki2, ki2[0:64, :], kiT[:, :])
                P.load(ki2, ki2[64:128, :], kiT[:, :])
                P.barrier()
                ek = 0
                lk = 0
                for ti in range(NC):
                    t0 = ti * 128
                    n = t0 + 128
                    qi_, q_, sg_, D_, b_ = qi_t[ti % 2], q_t[ti % 2], sg_t[ti % 2], Dall[ti % 2], bs[ti % 2]
                    P.load(qi_, qi_[:], qiT[:, t0:t0 + 128].rearrange("(c p) t -> p c t", p=128))
                    P.load(q_, q_[:], qrT[:, t0:t0 + 128].rearrange("(c p) t -> p c t", p=128))
                    P.load(sg_, sg_[:], sgn_d[t0:t0 + 128, :])
                    for h in range(8):
                        TS(P, 'pool' if h % 2 else 'dve', D_, D_[:, h, :], idb, idb[:], sg_[:, h:h + 1], None, ALU.mult, extra_r=(sg_,))
                    groups = [(s0, hp) for s0 in range(0, n, 512) for hp in range(4)]

                    def logits(g):
                        s0, hp = groups[g]
                        wdt = min(512, n - s0)
                        slot = (g % 2) * 2
                        for e2 in range(2):
                            h = hp * 2 + e2
                            p0 = (h % 2) * 64
                            oap = psum_t[:, slot + e2, 0:wdt]
                            la = qi_[p0:p0 + 64, h // 2, :]
                            ra = ki2[p0:p0 + 64, s0:s0 + wdt]
                            P.op('pe', lambda e, oap=oap, la=la, ra=ra: e.matmul(oap, lhsT=la, rhs=ra, start=True, stop=True),
                                 reads=(qi_, ki2), writes=(PS[slot], PS[slot + 1]))

                    logits(0)
                    for g in range(len(groups)):
                        s0, hp = groups[g]
                        wdt = min(512, n - s0)
                        slot = (g % 2) * 2
                        if g + 1 < len(groups):
                            logits(g + 1)
                        r_ = rl[lk % 3]
                        lk += 1
                        rin = psum_t[:, slot:slot + 2, 0:wdt]
                        P.op('act', lambda e, r_=r_, rin=rin, wdt=wdt: e.activation(out=r_[:, :, 0:wdt], in_=rin, func=AF.Relu),
                             reads=(PS[slot], PS[slot + 1]), writes=(r_,))
                        sb_, sa_ = bank(4 + (s0 // 512) % 2)
                        for e2 in range(2):
                            h = hp * 2 + e2
                            MM(P, sb_, sa_[:, 0:wdt], D_, D_[:, h, :], r_, r_[:, e2, 0:wdt], h == 0, h == 7)
                        if hp == 3:
                            CP(P, 'dve', Irow, Irow[:, s0:s0 + wdt], sb_, sa_[:, 0:wdt])
                    P.op('dve', lambda e: e.tensor_reduce(out=b_[:, 0:1], in_=Irow[:, 0:n], axis=AX.X, op=ALU.min), reads=(Irow,), writes=(b_,))
                    P.op('dve', lambda e: e.tensor_reduce(out=b_[:, 1:2], in_=Irow[:, 0:n], axis=AX.X, op=ALU.max), reads=(Irow,), writes=(b_,))
                    TT(P, 'dve', Irow, Irow[:, t0:n], Irow, Irow[:, t0:n], negm, negm[:], ALU.add)
                    TS(P, 'dve', b_, b_[:, 0:1], b_, b_[:, 0:1], -1.0, None, ALU.add)
                    TT(P, 'dve', b_, b_[:, 2:3], b_, b_[:, 1:2], b_, b_[:, 0:1], ALU.subtract)
                    TS(P, 'dve', b_, b_[:, 8:8 + NBIS + 1], cvec, cvec[:, 0:NBIS + 1], b_[:, 2:3], None, ALU.mult, extra_r=(b_,))
                    TT(P, 'dve', b_, b_[:, 3:4], b_, b_[:, 0:1], b_, b_[:, 8:9], ALU.add)
                    h0 = ((n // 2 + 127) // 128) * 128 if n > 128 else n
                    nB = n - h0
                    kk = min(256, S // 4) - 0.5
                    ba_ = bsa[ti % 2]
                    for it in range(NBIS):
                        if nB > 0:
                            ACT(P, junk2, junk2[:, 0:nB], Irow, Irow[:, h0:n], AF.Sign, bias=b_[:, 3:4], scale=-1.0,
                                accum=ba_[:, 0:1], extra_r=(b_,), extra_w=(ba_,))
                        TS(P, 'dve', junk, junk[:, 0:h0], Irow, Irow[:, 0:h0], b_[:, 3:4], None, ALU.is_ge, ALU.add,
                           accum=b_[:, 4:5], extra_r=(b_,), extra_w=(b_,))
                        if nB > 0:
                            STT(P, b_, b_[:, 4:5], ba_, ba_[:, 0:1], -0.5, b_, b_[:, 4:5], ALU.mult, ALU.add)
                        TS(P, 'dve', b_, b_[:, 5:6], b_, b_[:, 4:5], kk - nB / 2.0, 0.5, ALU.is_ge, ALU.subtract)
                        STT(P, b_, b_[:, 3:4], b_, b_[:, 5:6], b_[:, 8 + it:9 + it], b_, b_[:, 3:4], ALU.mult, ALU.add)
                    STT(P, b_, b_[:, 3:4], b_, b_[:, 8 + NBIS - 1:9 + NBIS - 1], -1.0001, b_, b_[:, 3:4], ALU.mult, ALU.add)
                    TS(P, 'dve', mrow, mrow[:, 0:n], Irow, Irow[:, 0:n], b_[:, 3:4], None, ALU.is_ge, extra_r=(b_,))
                    mT_ = mT[ti % 2]
                    for jg in range(0, ti + 1, 8):
                        nj = min(8, ti + 1 - jg)
                        pbk = PS[6 + (jg // 8) % 2]
                        pv = psum_t[:, 6 + (jg // 8) % 2, :].bitcast(BF16)
                        for jj in range(nj):
                            j = jg + jj
                            TR(P, pbk, pv[:, jj * 128:(jj + 1) * 128], mrow, mrow[:, j * 128:(j + 1) * 128], idb, idb[:])
                        CP(P, 'act', mT_, mT_[:, jg:jg + nj, :], pbk, pv[:, 0:nj * 128].rearrange("p (j t) -> p j t", j=nj))
                    def qk(j):
                        slot = (j % 2) * 4
                        kb_, vb_ = kb[j % 3], vb[j % 3]
                        P.load(kb_, kb_[:], kblk[j])
                        P.load(vb_, vb_[:], vblk[j])
                        for h in range(8):
                            p0 = (h % 2) * 64
                            oap = psum_t[:, slot + h % 2, (h // 2) * 128:(h // 2 + 1) * 128]
                            la = kb_[p0:p0 + 64, h // 2, :]
                            ra = q_[p0:p0 + 64, h // 2, :]
                            P.op('pe', lambda e, oap=oap, la=la, ra=ra: e.matmul(oap, lhsT=la, rhs=ra, start=True, stop=True),
                                 reads=(kb_, q_), writes=(PS[slot], PS[slot + 1]))

                    qk(0)
                    for j in range(ti + 1):
                        slot = (j % 2) * 4
                        vb_ = vb[j % 3]
                        if j + 1 <= ti:
                            qk(j + 1)
                        e_ = Eb[ek % 3]
                        ek += 1
                        ein = psum_t[:, slot:slot + 2, :].rearrange("p b (h t) -> p (b h) t", h=4)
                        P.op('act', lambda e, e_=e_, ein=ein: e.activation(out=e_[:], in_=ein, func=AF.Exp, scale=0.125),
                             reads=(PS[slot], PS[slot + 1]), writes=(e_,))
                        TT(P, 'dve', e_, e_[:], e_, e_[:], mT_, mT_[:, j:j + 1, :].to_broadcast([128, 8, 128]), ALU.mult)
                        for h in range(8):
                            oap = psum_t[:, 2 + h // 4, (h % 4) * 128:(h % 4 + 1) * 128]
                            la = vb_[:, h, :]
                            ra = e_[:, (h % 2) * 4 + h // 2, :]
                            st_ = (j == 0 and h % 4 == 0)
                            sp_ = (j == ti)
                            P.op('pe', lambda e, oap=oap, la=la, ra=ra, st_=st_, sp_=sp_: e.matmul(oap, lhsT=la, rhs=ra, start=st_, stop=sp_),
                                 reads=(vb_, e_), writes=(PS[2], PS[3]))
                    os_, rc_, yc_ = osb[ti % 2], rcp[ti % 2], yc[ti % 2]
                    oin = psum_t[:, 2:4, :].rearrange("p b (h t) -> p (b h) t", h=4)
                    P.op('dve', lambda e, os_=os_, oin=oin: e.tensor_copy(out=os_[:], in_=oin), reads=(PS[2], PS[3]), writes=(os_,))
                    o4 = os_[:].rearrange("p (c two) t -> p c two t", two=2)
                    r4 = rc_[:].rearrange("p (c two) t -> p c two t", two=2)
                    ACT(P, rc_, r4[0:64, :, 0, :], os_, o4[64:128, :, 0, :], AF.Copy)
                    ACT(P, rc_, r4[64:128, :, 1, :], os_, o4[0:64, :, 1, :], AF.Copy)
                    P.op('dve', lambda e: e.reciprocal(r4[0:64, :, 0, :], r4[0:64, :, 0, :]), reads=(rc_,), writes=(rc_,))
                    P.op('dve', lambda e: e.reciprocal(r4[64:128, :, 1, :], r4[64:128, :, 1, :]), reads=(rc_,), writes=(rc_,))
                    TT(P, 'dve', yc_, yc_[0:64, :, :], os_, o4[0:64, :, 0, :], rc_, r4[0:64, :, 0, :], ALU.mult)
                    TT(P, 'dve', yc_, yc_[64:128, :, :], os_, o4[64:128, :, 1, :], rc_, r4[64:128, :, 1, :], ALU.mult)
                    P.store(yc_, yT[1024:1536, t0:t0 + 128].rearrange("(c p) t -> p c t", p=128), yc_[:])
                P.barrier()

        def stage_merge(l):
            T = 512
            with contextlib.ExitStack() as es:
                wbr = P.sb(es, "e_wbr", [128, 12, D], BF16)
                wo = P.sb(es, "e_wo", [128, 8, D], BF16)
                P.load(wbr, wbr[:], wbr_b.rearrange("(c p) n -> p c n", p=128))
                P.load(wo, wo[:], wo_b.rearrange("(c p) n -> p c n", p=128))
                yt = [P.sb(es, f"e_y{i}", [128, 12, T], BF16) for i in range(2)]
                gt = [P.sb(es, f"e_g{i}", [128, 24, T], BF16) for i in range(2)]
                xt = [P.sb(es, f"e_x{i}", [128, 8, T], F32) for i in range(2)]
                mg = [P.sb(es, f"e_m{i}", [128, 8, T], BF16) for i in range(2)]
                acc = [P.sb(es, f"e_a{i}", [128, T], F32) for i in range(2)]
                tmp = [P.sb(es, f"e_t{i}", [128, T], F32) for i in range(2)]
                k = 0
                for ti, t0 in enumerate(range(0, S, T)):
                    y_, g_, x_, m_ = yt[ti % 2], gt[ti % 2], xt[ti % 2], mg[ti % 2]
                    P.load(y_, y_[:], yT[:, t0:t0 + T].rearrange("(c p) t -> p c t", p=128))
                    P.load(g_, g_[:], fmT[FM_G:FM_G + 3072, t0:t0 + T].rearrange("(c p) t -> p c t", p=128))
                    P.load(x_, x_[:], xT[:, t0:t0 + T].rearrange("(c p) t -> p c t", p=128))
                    ACT(P, g_, g_[:], g_, g_[:], AF.Sigmoid)
                    for nn in range(8):
                        a_, t_ = acc[nn % 2], tmp[nn % 2]
                        for br in range(3):
                            pb, pa = bank((k % 2) * 3 + br)
                            for c in range(4):
                                MM(P, pb, pa, wbr, wbr[:, br * 4 + c, nn * 128:(nn + 1) * 128], y_, y_[:, br * 4 + c, :], c == 0, c == 3)
                            if br == 0:
                                TT(P, 'dve', a_, a_[:], pb, pa, g_, g_[:, nn, :], ALU.mult)
                            else:
                                TT(P, 'dve', t_, t_[:], pb, pa, g_, g_[:, br * 8 + nn, :], ALU.mult)
                                if br == 1:
                                    TT(P, 'pool', a_, a_[:], a_, a_[:], t_, t_[:], ALU.add)
                                else:
                                    TT(P, 'dve', m_, m_[:, nn, :], a_, a_[:], t_, t_[:], ALU.add)
                        k += 1
                    for nn in range(8):
                        pb, pa = bank(6 + nn % 2)
                        for c in range(8):
                            MM(P, pb, pa, wo, wo[:, c, nn * 128:(nn + 1) * 128], m_, m_[:, c, :], c == 0, c == 7)
                        TT(P, 'dve', x_, x_[:, nn, :], x_, x_[:, nn, :], pb, pa, ALU.add)
                    P.store(x_, xT[:, t0:t0 + T].rearrange("(c p) t -> p c t", p=128), x_[:])
                P.barrier()

        def stage_ffn(l):
            T = 512
            with contextlib.ExitStack() as es:
                nb = (P.sb(es, "f_xt", [128, 8, T], F32), P.sb(es, "f_sq", [128, 8, T], BF16),
                      P.sb(es, "f_hT", [128, 8, T], BF16), P.sb(es, "f_rstd", [128, T], F32))
                wu = [P.sb(es, f"f_wu{i}", [128, 8, 512], BF16) for i in range(3)]
                wd = [P.sb(es, f"f_wd{i}", [128, 24, 256], BF16) for i in range(2)]
                cw = P.sb(es, "f_cw", [128, 24, 3], F32)
                cb = P.sb(es, "f_cb", [128, 24], F32)
                G = [P.sb(es, f"f_G{i}", [128, T + 2], F32) for i in range(3)]
                carry = P.sb(es, "f_carry", [128, 24, 2], F32)
                accs = [P.sb(es, f"f_acc{i}", [128, T], F32) for i in range(3)]
                at = P.sb(es, "f_at", [128, 24, T], BF16)
                P.load(cw, cw[:], fcw_in[l])
                P.load(cb, cb[:], fcb_in[l])
                MEMSET(P, 'dve', carry, carry[:], 0.0)
                wk = 0
                gk = 0
                for t0 in range(0, S, T):
                    xt, hT = rmsnorm_tile(nb, t0, T)
                    for blk in range(6):
                        wg_, wu_ = wu[wk % 3], wu[(wk + 1) % 3]
                        wk += 2
                        P.load(wg_, wg_[:], fup_b[:, blk * 512:(blk + 1) * 512].rearrange("(c p) n -> p c n", p=128))
                        P.load(wu_, wu_[:], fup_b[:, DFF + blk * 512:DFF + (blk + 1) * 512].rearrange("(c p) n -> p c n", p=128))
                        for ch in range(4):
                            f = blk * 4 + ch
                            pbg, pag = bank((gk % 3) * 2)
                            pbu, pau = bank((gk % 3) * 2 + 1)
                            for c in range(8):
                                MM(P, pbg, pag, wg_, wg_[:, c, ch * 128:(ch + 1) * 128], hT, hT[:, c, :], c == 0, c == 7)
                            for c in range(8):
                                MM(P, pbu, pau, wu_, wu_[:, c, ch * 128:(ch + 1) * 128], hT, hT[:, c, :], c == 0, c == 7)
                            g_, a_ = G[gk % 3], accs[gk % 3]
                            gk += 1
                            CP(P, 'dve', g_, g_[:, 0:2], carry, carry[:, f, :])
                            CP(P, 'act', g_, g_[:, 2:2 + T], pbg, pag)
                            CP(P, 'dve', carry, carry[:, f, :], g_, g_[:, T:T + 2])
                            TS(P, 'dve', a_, a_[:], g_, g_[:, 0:T], cw[:, f, 0:1], None, ALU.mult, extra_r=(cw,))
                            STT(P, a_, a_[:], g_, g_[:, 1:T + 1], cw[:, f, 1:2], a_, a_[:], ALU.mult, ALU.add, extra_r=(cw,))
                            STT(P, a_, a_[:], g_, g_[:, 2:T + 2], cw[:, f, 2:3], a_, a_[:], ALU.mult, ALU.add, extra_r=(cw,))
                            ACT(P, a_, a_[:], a_, a_[:], AF.Gelu_apprx_tanh, bias=cb[:, f:f + 1], extra_r=(cb,))
                            TT(P, 'dve', at, at[:, f, :], a_, a_[:], pbu, pau, ALU.mult)
                    for half in range(4):
                        w_ = wd[half % 2]
                        P.load(w_, w_[:], fdn_b[:, half * 256:(half + 1) * 256].rearrange("(c p) n -> p c n", p=128))
                        for n2 in range(2):
                            nn = half * 2 + n2
                            pb, pa = bank(6 + nn % 2)
                            for c in range(24):
                                MM(P, pb, pa, w_, w_[:, c, n2 * 128:(n2 + 1) * 128], at, at[:, c, :], c == 0, c == 23)
                            TT(P, 'dve', xt, xt[:, nn, :T], xt, xt[:, nn, :T], pb, pa, ALU.add)
                    P.store(xt, xT[:, t0:t0 + T].rearrange("(c p) t -> p c t", p=128), xt[:, :, :T])
                P.barrier()

        def stage_final():
            T = 512
            with contextlib.ExitStack() as es:
                nb = (P.sb(es, "z_xt", [128, 8, T], F32), P.sb(es, "z_sq", [128, 8, T], BF16),
                      P.sb(es, "z_hT", [128, 8, T], F32), P.sb(es, "z_rstd", [128, T], F32))
                gc = P.sb(es, "z_g", [128, 8], F32)
                P.load(gc, gc[:], gfin_in)
                ob = [P.sb(es, f"z_o{i}", [128, D], F32) for i in range(2)]
                k = 0
                for t0 in range(0, S, T):
                    xt, hT = rmsnorm_tile(nb, t0, T, gcol=gc)
                    for st in range(4):
                        o = ob[k % 2]
                        for hlf in range(2):
                            pb, pa = bank((k % 2) * 2 + hlf)
                            for c in range(4):
                                cc = hlf * 4 + c
                                TR(P, pb, pa[:, c * 128:(c + 1) * 128], hT, hT[:, cc, st * 128:(st + 1) * 128], idf, idf[:])
                            CP(P, 'dve' if hlf == 0 else 'act', o, o[:, hlf * 512:(hlf + 1) * 512], pb, pa)
                        k += 1
                        P.store(o, out_d[t0 + st * 128:t0 + (st + 1) * 128, :], o[:])
                P.barrier()

        stage_transpose_in()
        if stop >= 1:
            stage_rope_tables()
        for l in range(depth):
            if stop >= 2:
                convert("wA", wA_in[l], wA_b, D, NA, gmix_in[l])
                convert("wbr", wbr_in[l], wbr_b, 1536, D)
                convert("wo", wo_in[l], wo_b, D, D)
                convert("fup", fup_in[l], fup_b, D, 2 * DFF, gffn_in[l])
                convert("fdn", fdn_in[l], fdn_b, DFF, D)
            if stop >= 3:
                stage_inproj()
            if stop >= 4:
                stage_mlstm(l)
            if stop >= 5:
                stage_rglru(l)
            if stop >= 6:
                stage_rope(l)
            if stop >= 7:
                stage_dsa(l)
            if stop >= 8:
                stage_merge(l)
            if stop >= 9:
                stage_ffn(l)
        if stop >= 10:
            stage_final()
    print("ninst", P.ninst, "nwait", P.nwait, "pool", len(P.pool))
    return nc


def make_consts():
    bf = ml_dtypes.bfloat16
    idf = np.eye(128, dtype=np.float32)
    s = np.arange(128)
    maskT = (s[:, None] <= s[None, :]).astype(np.float32)
    negm = np.where(s[None, :] <= s[:, None], 0.0, -1e30).astype(np.float32)
    rot = np.zeros((128, 128), np.float32)
    for m in range(128):
        hm, dm = divmod(m, 64)
        if dm < 32:
            rot[hm * 64 + dm + 32, m] = -1.0
        else:
            rot[hm * 64 + dm - 32, m] = 1.0
    sel8 = np.zeros((8, 4, 128), np.float32)
    for ch in range(4):
        for m in range(128):
            sel8[2 * ch + (m >= 64), ch, m] = 1.0
    selh = np.zeros((4, 4, 128), np.float32)
    for h in range(4):
        selh[h, h, :] = 1.0
    cvec = np.tile((2.0 ** -(np.arange(16) + 1.0))[None, :], (128, 1)).astype(np.float32)
    invf = (10000.0 ** (-(2.0 * (np.arange(128) % 32)) / 64.0)).astype(np.float32)[:, None]
    return dict(c_idf=idf, c_idb=idf.astype(bf), c_ones=np.ones((128, 128), bf), c_maskT=maskT.astype(bf),
                c_negm=negm, c_rot=rot.astype(bf), c_id240=(240.0 * idf).astype(bf), c_sel8=sel8, c_selh=selh, c_cvec=cvec, c_invf=invf)


def colmajor(v, nchunk):
    v = np.asarray(v, np.float32)
    return np.ascontiguousarray(np.swapaxes(v.reshape(v.shape[:-1] + (nchunk, 128)), -1, -2))


def prep_shared(inp, depth):
    f = np.float32
    fm, small, tm = perm_w_in()
    w_in = np.asarray(inp['w_in'], f)[:depth]
    wA = np.zeros((depth, D, NA), f)
    wA[:, :, 0:6656] = w_in[:, :, fm]
    wA[:, :, 6656:6656 + len(small)] = w_in[:, :, small]
    wA[:, :, 6784:] = w_in[:, :, tm]

    def bd(w):
        o = np.zeros((depth, 4, 128, 128), f)
        w = np.asarray(w, f)[:depth]
        for n in range(8):
            o[:, n // 2, (n % 2) * 64:(n % 2 + 1) * 64, (n % 2) * 64:(n % 2 + 1) * 64] = w[:, n]
        return o

    def chcol(v, nch):
        return np.ascontiguousarray(np.asarray(v, f)[:depth].reshape(depth, nch, 128, 1))

    def chtap(w, nch):
        w = np.asarray(w, f)[:depth]
        K = w.shape[1]
        return np.ascontiguousarray(w.reshape(depth, K, nch, 128).transpose(0, 2, 3, 1))

    d = dict(
        wA=wA, gmix=colmajor(inp['norm_mix'][:depth], 8), gffn=colmajor(inp['norm_ffn'][:depth], 8),
        gfin=colmajor(inp['norm_final'], 8),
        mcw=chtap(inp['mlstm_conv_w'], 8), mcb=chcol(inp['mlstm_conv_b'], 8),
        mib=np.asarray(inp['mlstm_i_bias'], f)[:depth].reshape(depth, 4, 1),
        mfb=np.asarray(inp['mlstm_f_bias'], f)[:depth].reshape(depth, 4, 1),
        mng=np.asarray(inp['mlstm_norm'], f)[:depth].reshape(depth, 4, 128),
        rcw=chtap(inp['rglru_conv_w'], 4), rcb=chcol(inp['rglru_conv_b'], 4),
        rwa=bd(inp['rglru_w_a']), rwx=bd(inp['rglru_w_x']),
        rba=chcol(inp['rglru_b_a'], 4), rbx=chcol(inp['rglru_b_x'], 4), rlam=chcol(inp['rglru_lambda'], 4),
        wbr=np.ascontiguousarray(np.asarray(inp['w_branch'], f)[:depth].reshape(depth, 1536, D)),
        wo=np.asarray(inp['w_out'], f)[:depth], fup=np.asarray(inp['ffn_up'], f)[:depth],
        fcw=np.ascontiguousarray(chtap(inp['ffn_conv_w'], 24).transpose(0, 2, 1, 3)), fcb=colmajor(np.asarray(inp['ffn_conv_b'])[:depth], 24),
        fdn=np.asarray(inp['ffn_down'], f)[:depth],
    )
    d.update(make_consts())
    return d


def kernel(**inputs):
    x = np.asarray(inputs['x'], np.float32)
    pos = np.asarray(inputs['positions'], np.int32)
    B, S, _ = x.shape
    depth = inputs['w_in'].shape[0]
    nc = build(S, depth)
    shared = prep_shared(inputs, depth)
    in_maps = []
    for c in range(8):
        b = c % B
        m = dict(shared)
        m['x'] = np.ascontiguousarray(x[b])
        m['pos'] = np.ascontiguousarray(pos[b:b + 1])
        in_maps.append(m)
    res = run_bass_kernel_spmd(nc, in_maps, core_ids=list(range(8)))
    return np.stack([np.asarray(res.results[b]['out'], np.float32) for b in range(B)], axis=0)
```
